# Optimizing a Trainium2 kernel written in Bass

```python
import jax, jax.numpy as jnp
from jax import lax
import numpy as np

D_MODEL = 1024
BATCH = 16
SEQ = 2048
DEPTH = 2

GRID_W = 64
CTX_LEN = 256
N_HEADS_A = 8
N_KV_A = 2
HEAD_DIM_A = 64
WINDOW = 128
MLA_HEADS = 8
MLA_Q_RANK = 384
MLA_KV_RANK = 256
MLA_NOPE = 64
MLA_ROPE = 32
MLA_V = 64
N_HEADS_C = 8
N_KV_C = 2
HEAD_DIM_C = 128
N_EXPERTS = 32
TOP_K = 4
D_FF = 1024
SWIGLU_ALPHA = 1.702
SWIGLU_LIMIT = 7.0
Q_BLOCK = 128
EXPERT_BLOCK = 128
ROPE_THETA = 10000.0
RMS_EPS = 1e-6
NEG_INF = -1e30
N_EVEN = (DEPTH + 1) // 2
N_ODD = DEPTH // 2
AB_SPLIT_SIZES = [N_HEADS_A * HEAD_DIM_A, N_KV_A * HEAD_DIM_A, N_KV_A * HEAD_DIM_A,
                  MLA_Q_RANK, MLA_KV_RANK, MLA_ROPE]
AB_IN = sum(AB_SPLIT_SIZES)
AB_OUT = N_HEADS_A * HEAD_DIM_A + MLA_HEADS * MLA_V
C_IN = (N_HEADS_C + 2 * N_KV_C) * HEAD_DIM_C
C_OUT = N_HEADS_C * HEAD_DIM_C

kernel_name = "hybrid_swa_mla_gqa2d_moe_dit"


def rms_norm(x, g):
    xf = x.astype(jnp.float32)
    y = xf * lax.rsqrt(jnp.mean(xf * xf, axis=-1, keepdims=True) + RMS_EPS)
    return (y * g.astype(jnp.float32)).astype(x.dtype)


def grid_positions(n_tokens):
    n_rows = n_tokens // GRID_W
    rows = jnp.broadcast_to(jnp.arange(n_rows, dtype=jnp.int32)[:, None], (n_rows, GRID_W)).reshape(-1)
    cols = jnp.broadcast_to(jnp.arange(GRID_W, dtype=jnp.int32)[None, :], (n_rows, GRID_W)).reshape(-1)
    return rows, cols


def axial_rope_table(rows, cols, rot_dim):
    quarter = rot_dim // 4
    inv = ROPE_THETA ** (-jnp.arange(quarter, dtype=jnp.float32) / quarter)
    ang = jnp.concatenate([rows.astype(jnp.float32)[:, None] * inv,
                           cols.astype(jnp.float32)[:, None] * inv], axis=-1)
    return jnp.cos(ang), jnp.sin(ang)


def apply_rope(x, table):
    cos, sin = table
    cos = cos.astype(x.dtype)
    sin = sin.astype(x.dtype)
    x1, x2 = jnp.split(x, 2, axis=-1)
    return jnp.concatenate([x1 * cos - x2 * sin, x2 * cos + x1 * sin], axis=-1)


def heads_to_tokens(o):
    b, hk, g, t, d = o.shape
    return o.transpose(0, 3, 1, 2, 4).reshape(b, t, hk * g * d)


def sdpa(q, k, v, mask=None, sink=None):
    s = jnp.einsum('bhgqd,bhkd->bhgqk', q, k, preferred_element_type=jnp.float32)
    if mask is not None:
        s = jnp.where(mask, s, NEG_INF)
    if sink is None:
        p = jax.nn.softmax(s, axis=-1)
    else:
        sk = sink.astype(jnp.float32)[None, :, :, None, None]
        m = jnp.maximum(jnp.max(s, axis=-1, keepdims=True), sk)
        e = jnp.exp(s - m)
        p = e / (jnp.sum(e, axis=-1, keepdims=True) + jnp.exp(sk - m))
    return jnp.einsum('bhgqk,bhkd->bhgqd', p.astype(v.dtype), v)


def dense_block_attention(q, k, v):
    b, hk, g, t, _ = q.shape
    nb = t // Q_BLOCK

    def one(i):
        qb = lax.dynamic_slice_in_dim(q, i * Q_BLOCK, Q_BLOCK, axis=3)
        return sdpa(qb, k, v)

    out = lax.map(one, jnp.arange(nb))
    return jnp.moveaxis(out, 0, 3).reshape(b, hk, g, t, v.shape[-1])


def window_block_attention(q, k, v, k_ctx, v_ctx, sink):
    b, hk, g, t, _ = q.shape
    nb = t // Q_BLOCK
    pad = ((0, 0), (0, 0), (Q_BLOCK, Q_BLOCK), (0, 0))
    kp = jnp.pad(k, pad)
    vp = jnp.pad(v, pad)
    qi = jnp.arange(Q_BLOCK, dtype=jnp.int32)[:, None]
    kj = jnp.arange(3 * Q_BLOCK, dtype=jnp.int32)[None, :]
    ctx_ok = jnp.ones((Q_BLOCK, k_ctx.shape[2]), dtype=bool)

    def one(i):
        qb = lax.dynamic_slice_in_dim(q, i * Q_BLOCK, Q_BLOCK, axis=3)
        kb = lax.dynamic_slice_in_dim(kp, i * Q_BLOCK, 3 * Q_BLOCK, axis=2)
        vb = lax.dynamic_slice_in_dim(vp, i * Q_BLOCK, 3 * Q_BLOCK, axis=2)
        qpos = i * Q_BLOCK + qi
        kpos = (i - 1) * Q_BLOCK + kj
        ok = (jnp.abs(kpos - qpos) <= WINDOW) & (kpos >= 0) & (kpos < t)
        mask = jnp.concatenate([ok, ctx_ok], axis=-1)
        return sdpa(qb, jnp.concatenate([kb, k_ctx], axis=2), jnp.concatenate([vb, v_ctx], axis=2), mask, sink)

    out = lax.map(one, jnp.arange(nb))
    return jnp.moveaxis(out, 0, 3).reshape(b, hk, g, t, v.shape[-1])


def ab_heads(h, w_in, q_g, wq_b, kv_g, wkv_b, rope_a, rope_b):
    bn, t, _ = h.shape
    splits = np.cumsum(AB_SPLIT_SIZES)[:-1].tolist()
    qa, ka, va, cq, ckv, kr = jnp.split(h @ w_in, splits, axis=-1)
    qa = qa.reshape(bn, t, N_KV_A, N_HEADS_A // N_KV_A, HEAD_DIM_A).transpose(0, 2, 3, 1, 4)
    ka = ka.reshape(bn, t, N_KV_A, HEAD_DIM_A).transpose(0, 2, 1, 3)
    va = va.reshape(bn, t, N_KV_A, HEAD_DIM_A).transpose(0, 2, 1, 3)
    qb = (rms_norm(cq, q_g) @ wq_b).reshape(bn, t, MLA_HEADS, MLA_NOPE + MLA_ROPE).transpose(0, 2, 1, 3)
    kvb = (rms_norm(ckv, kv_g) @ wkv_b).reshape(bn, t, MLA_HEADS, MLA_NOPE + MLA_V).transpose(0, 2, 1, 3)
    q_nope, q_rope = qb[..., :MLA_NOPE], qb[..., MLA_NOPE:]
    k_nope, vb = kvb[..., :MLA_NOPE], kvb[..., MLA_NOPE:]
    kr = kr[:, None]
    if rope_a is not None:
        qa = apply_rope(qa, rope_a)
        ka = apply_rope(ka, rope_a)
        q_rope = apply_rope(q_rope, rope_b)
        kr = apply_rope(kr, rope_b)
    qa = qa * HEAD_DIM_A ** -0.5
    qb = jnp.concatenate([q_nope, q_rope], axis=-1)[:, :, None] * (MLA_NOPE + MLA_ROPE) ** -0.5
    kb = jnp.concatenate([k_nope, jnp.broadcast_to(kr, k_nope.shape[:-1] + (MLA_ROPE,))], axis=-1)
    return qa, ka, va, qb, kb, vb


def mixer_ab(h, hc, w_in, q_g, wq_b, kv_g, wkv_b, sink, w_out, rope_a, rope_b, with_ctx_out):
    qa, ka, va, qb, kb, vb = ab_heads(h, w_in, q_g, wq_b, kv_g, wkv_b, rope_a, rope_b)
    qa_c, ka_c, va_c, qb_c, kb_c, vb_c = ab_heads(hc, w_in, q_g, wq_b, kv_g, wkv_b, None, None)
    sink = sink.reshape(N_KV_A, N_HEADS_A // N_KV_A)
    oa = window_block_attention(qa, ka, va, ka_c, va_c, sink)
    ob = dense_block_attention(qb, jnp.concatenate([kb, kb_c], axis=2), jnp.concatenate([vb, vb_c], axis=2))
    y = jnp.concatenate([heads_to_tokens(oa), heads_to_tokens(ob)], axis=-1) @ w_out
    yc = None
    if with_ctx_out:
        oa_c = sdpa(qa_c, ka_c, va_c, None, sink)
        ob_c = sdpa(qb_c, kb_c, vb_c)
        yc = jnp.concatenate([heads_to_tokens(oa_c), heads_to_tokens(ob_c)], axis=-1) @ w_out
    return y, yc


def c_heads(h, w_in, qn_g, kn_g, rope):
    bn, t, _ = h.shape
    q, k, v = jnp.split(h @ w_in, [N_HEADS_C * HEAD_DIM_C, (N_HEADS_C + N_KV_C) * HEAD_DIM_C], axis=-1)
    q = rms_norm(q.reshape(bn, t, N_KV_C, N_HEADS_C // N_KV_C, HEAD_DIM_C), qn_g).transpose(0, 2, 3, 1, 4)
    k = rms_norm(k.reshape(bn, t, N_KV_C, HEAD_DIM_C), kn_g).transpose(0, 2, 1, 3)
    v = v.reshape(bn, t, N_KV_C, HEAD_DIM_C).transpose(0, 2, 1, 3)
    if rope is not None:
        q = apply_rope(q, rope)
        k = apply_rope(k, rope)
    return q * HEAD_DIM_C ** -0.5, k, v


def mixer_c(h, hc, w_in, qn_g, kn_g, w_out, rope_c, with_ctx_out):
    q, k, v = c_heads(h, w_in, qn_g, kn_g, rope_c)
    q_c, k_c, v_c = c_heads(hc, w_in, qn_g, kn_g, None)
    o = dense_block_attention(q, jnp.concatenate([k, k_c], axis=2), jnp.concatenate([v, v_c], axis=2))
    y = heads_to_tokens(o) @ w_out
    yc = None
    if with_ctx_out:
        yc = heads_to_tokens(sdpa(q_c, k_c, v_c)) @ w_out
    return y, yc


def clamped_swiglu(u):
    glu = jnp.minimum(u[..., ::2], SWIGLU_LIMIT)
    lin = jnp.clip(u[..., 1::2], -SWIGLU_LIMIT, SWIGLU_LIMIT)
    return glu * jax.nn.sigmoid(SWIGLU_ALPHA * glu) * (lin + 1)


def moe(h, router_w, router_b, w_in, b_in, w_out, b_out):
    n_tok, d = h.shape
    logits = (h @ router_w + router_b).astype(jnp.float32)
    top_vals, top_idx = lax.top_k(logits, TOP_K)
    gates = jax.nn.softmax(top_vals, axis=-1)
    n_assign = n_tok * TOP_K
    e_flat = top_idx.reshape(-1).astype(jnp.int32)
    tok_flat = jnp.arange(n_assign, dtype=jnp.int32) // TOP_K
    g_flat = gates.reshape(-1)
    order = jnp.argsort(e_flat)
    e_s, tok_s, g_s = e_flat[order], tok_flat[order], g_flat[order]
    counts = jnp.bincount(e_flat, length=N_EXPERTS).astype(jnp.int32)
    starts = jnp.cumsum(counts) - counts
    padded = (counts + EXPERT_BLOCK - 1) // EXPERT_BLOCK * EXPERT_BLOCK
    pends = jnp.cumsum(padded)
    pstarts = pends - padded
    dest = pstarts[e_s] + (jnp.arange(n_assign, dtype=jnp.int32) - starts[e_s])
    n_blocks = (n_assign + N_EXPERTS * (EXPERT_BLOCK - 1) + EXPERT_BLOCK - 1) // EXPERT_BLOCK
    cap = n_blocks * EXPERT_BLOCK
    tok_buf = jnp.zeros((cap,), jnp.int32).at[dest].set(tok_s)
    g_buf = jnp.zeros((cap,), h.dtype).at[dest].set(g_s.astype(h.dtype))
    blk_start = jnp.arange(n_blocks, dtype=jnp.int32) * EXPERT_BLOCK
    blk_expert = jnp.minimum(jnp.searchsorted(pends, blk_start, side='right'), N_EXPERTS - 1)

    def one(args):
        toks, e = args
        u = h[toks] @ w_in[e] + b_in[e]
        return clamped_swiglu(u) @ w_out[e] + b_out[e]

    y = lax.map(one, (tok_buf.reshape(n_blocks, EXPERT_BLOCK), blk_expert)).reshape(cap, d)
    return jnp.zeros_like(h).at[tok_buf].add(y * g_buf[:, None])


def setup_inputs(seed: int = 0) -> dict:
    key = jax.random.key(seed)
    ks = iter(jax.random.split(key, 32))

    def nrm(shape, scale):
        return jax.random.normal(next(ks), shape, jnp.float32) * scale

    def gain(shape):
        return 1.0 + nrm(shape, 0.05)

    return {
        "x": nrm((BATCH, SEQ, D_MODEL), 1.0),
        "c": nrm((BATCH, D_MODEL), 1.0),
        "ctx": nrm((BATCH, CTX_LEN, D_MODEL), 1.0),
        "c_ctx": nrm((D_MODEL,), 1.0),
        "ada_w": nrm((DEPTH, D_MODEL, 6 * D_MODEL), 0.5 * D_MODEL ** -0.5),
        "ada_b": nrm((DEPTH, 6 * D_MODEL), 0.02),
        "norm_mix_g": gain((DEPTH, D_MODEL)),
        "norm_ffn_g": gain((DEPTH, D_MODEL)),
        "ab_w_in": nrm((N_EVEN, D_MODEL, AB_IN), D_MODEL ** -0.5),
        "mla_q_norm_g": gain((N_EVEN, MLA_Q_RANK)),
        "mla_wq_b": nrm((N_EVEN, MLA_Q_RANK, MLA_HEADS * (MLA_NOPE + MLA_ROPE)), MLA_Q_RANK ** -0.5),
        "mla_kv_norm_g": gain((N_EVEN, MLA_KV_RANK)),
        "mla_wkv_b": nrm((N_EVEN, MLA_KV_RANK, MLA_HEADS * (MLA_NOPE + MLA_V)), MLA_KV_RANK ** -0.5),
        "swa_sink": nrm((N_EVEN, N_HEADS_A), 0.5),
        "ab_w_out": nrm((N_EVEN, AB_OUT, D_MODEL), AB_OUT ** -0.5),
        "c_w_in": nrm((N_ODD, D_MODEL, C_IN), D_MODEL ** -0.5),
        "c_q_norm_g": gain((N_ODD, HEAD_DIM_C)),
        "c_k_norm_g": gain((N_ODD, HEAD_DIM_C)),
        "c_w_out": nrm((N_ODD, C_OUT, D_MODEL), C_OUT ** -0.5),
        "router_w": nrm((DEPTH, D_MODEL, N_EXPERTS), D_MODEL ** -0.5),
        "router_b": nrm((DEPTH, N_EXPERTS), 0.01),
        "moe_w_in": nrm((DEPTH, N_EXPERTS, D_MODEL, 2 * D_FF), D_MODEL ** -0.5),
        "moe_b_in": nrm((DEPTH, N_EXPERTS, 2 * D_FF), 0.02),
        "moe_w_out": nrm((DEPTH, N_EXPERTS, D_FF, D_MODEL), D_FF ** -0.5),
        "moe_b_out": nrm((DEPTH, N_EXPERTS, D_MODEL), 0.02),
        "final_norm_g": gain((D_MODEL,)),
    }


def reference(x, c, ctx, c_ctx, ada_w, ada_b, norm_mix_g, norm_ffn_g, ab_w_in, mla_q_norm_g, mla_wq_b,
              mla_kv_norm_g, mla_wkv_b, swa_sink, ab_w_out, c_w_in, c_q_norm_g, c_k_norm_g, c_w_out,
              router_w, router_b, moe_w_in, moe_b_in, moe_w_out, moe_b_out, final_norm_g):
    bn, s, d = x.shape
    n_ctx = ctx.shape[1]
    rows, cols = grid_positions(s)
    rope_a = axial_rope_table(rows, cols, HEAD_DIM_A)
    rope_b = axial_rope_table(rows, cols, MLA_ROPE)
    rope_c = axial_rope_table(rows, cols, HEAD_DIM_C)
    c_act = jax.nn.silu(c)
    cc_act = jax.nn.silu(c_ctx)
    for i in range(DEPTH):
        with_ctx = i < DEPTH - 1
        j = i // 2
        mod = c_act @ ada_w[i] + ada_b[i]
        mod_c = cc_act @ ada_w[i] + ada_b[i]
        sh1, sc1, g1, sh2, sc2, g2 = [m[:, None] for m in jnp.split(mod, 6, axis=-1)]
        sh1c, sc1c, g1c, sh2c, sc2c, g2c = jnp.split(mod_c, 6, axis=-1)
        h = rms_norm(x, norm_mix_g[i]) * (1 + sc1) + sh1
        hc = rms_norm(ctx, norm_mix_g[i]) * (1 + sc1c) + sh1c
        if i % 2 == 0:
            y, yc = mixer_ab(h, hc, ab_w_in[j], mla_q_norm_g[j], mla_wq_b[j], mla_kv_norm_g[j], mla_wkv_b[j],
                             swa_sink[j], ab_w_out[j], rope_a, rope_b, with_ctx)
        else:
            y, yc = mixer_c(h, hc, c_w_in[j], c_q_norm_g[j], c_k_norm_g[j], c_w_out[j], rope_c, with_ctx)
        x = x + g1 * y
        tokens = (rms_norm(x, norm_ffn_g[i]) * (1 + sc2) + sh2).reshape(bn * s, d)
        if with_ctx:
            ctx = ctx + g1c * yc
            hc2 = rms_norm(ctx, norm_ffn_g[i]) * (1 + sc2c) + sh2c
            tokens = jnp.concatenate([tokens, hc2.reshape(bn * n_ctx, d)], axis=0)
        f = moe(tokens, router_w[i], router_b[i], moe_w_in[i], moe_b_in[i], moe_w_out[i], moe_b_out[i])
        x = x + g2 * f[:bn * s].reshape(bn, s, d)
        if with_ctx:
            ctx = ctx + g2c * f[bn * s:].reshape(bn, n_ctx, d)
    return rms_norm(x, final_norm_g)
```

```python
import concourse.bass as bass
import concourse.mybir as mybir

F32 = mybir.dt.float32
BF16 = mybir.dt.bfloat16
I32 = mybir.dt.int32
U32 = mybir.dt.uint32
ALU = mybir.AluOpType
AF = mybir.ActivationFunctionType
AX = mybir.AxisListType


class _Op:
    __slots__ = ("eng", "fn", "deps", "dma", "needed", "sig", "idx", "thr", "cond", "key")

    def __init__(self, eng, fn, dma):
        self.cond = None
        self.key = None
        self.eng = eng
        self.fn = fn
        self.dma = dma
        self.deps = []
        self.needed = dma
        self.sig = None
        self.thr = None


class Sched:
    ENGS = ("pe", "act", "dve", "pool", "sp")
    KDMA = 8

    def __init__(self, nc):
        self.nc = nc
        self.ops = []
        self.lastw = {}
        self.readers = {}
        self.cur_cond = None
        self.cnt_ap = None
        self.cnt_dep = None
        self.key = None
        self.dbl = set()
        self.sfx = ""

    def _add(self, eng, fn, reads, writes, dma):
        if self.dbl:
            reads = tuple(r + self.sfx if r in self.dbl else r for r in reads)
            writes = tuple(r + self.sfx if r in self.dbl else r for r in writes)
        op = _Op(eng, fn, dma)
        deps = {}
        for r in reads:
            w = self.lastw.get(r)
            if w is not None:
                deps[id(w)] = w
        for r in writes:
            w = self.lastw.get(r)
            if w is not None:
                deps[id(w)] = w
            for rd in self.readers.get(r, ()):
                deps[id(rd)] = rd
        for r in writes:
            self.lastw[r] = op
            self.readers[r] = []
        for r in reads:
            if r not in writes:
                self.readers.setdefault(r, []).append(op)
        for d in deps.values():
            if d is op:
                continue
            if d.eng == "pe" and eng == "pe" and not d.dma:
                continue
            d.needed = True
            op.deps.append(d)
        op.cond = self.cur_cond
        op.key = self.key
        self.ops.append(op)
        return op

    def op(self, eng, fn, reads=(), writes=()):
        return self._add(eng, fn, tuple(reads), tuple(writes), False)

    def dma(self, queue, fn, reads=(), writes=()):
        return self._add(queue, fn, tuple(reads), tuple(writes), True)

    def emit(self):
        nc = self.nc
        from contextlib import ExitStack

        with ExitStack() as es:
            Sched._uid = getattr(Sched, "_uid", 0) + 1
            u = "p%d_" % Sched._uid
            esem = {e: nc.alloc_semaphore(name=u + "s_" + e) for e in self.ENGS}
            qsem = {
                q: [nc.alloc_semaphore(name=u + "d_%s%d" % (q, i)) for i in range(self.KDMA)]
                for q in ("sp", "pool")
            }
            ecount = {e: 0 for e in self.ENGS}
            qn = {q: 0 for q in qsem}
            per = {e: [o for o in self.ops if o.eng == e] for e in self.ENGS}
            if any(o.key is not None for o in self.ops):
                seen = False
                last = max(i for i, o in enumerate(self.ops) if o.key is not None)
                sk = {}
                for i, o in enumerate(self.ops):
                    if o.key is not None:
                        seen = True
                        sk[id(o)] = float(o.key[0] + o.key[1])
                    else:
                        sk[id(o)] = float("inf") if (seen and i > last) else (float("-inf") if not seen else None)
                        assert sk[id(o)] is not None, "unkeyed op inside a keyed loop"
                groups = {}
                for o in self.ops:
                    groups.setdefault(sk[id(o)], []).append(o)
                glob = []
                for k in sorted(groups):
                    g = groups[k]
                    tiles = []
                    for o in g:
                        t = o.key[0] if o.key is not None else None
                        if t not in tiles:
                            tiles.append(t)
                    if len(tiles) <= 1:
                        glob.extend(g)
                        continue
                    items = []
                    for ti, t in enumerate(tiles):
                        seq = [o for o in g if (o.key[0] if o.key is not None else None) == t]
                        for i, o in enumerate(seq):
                            items.append(((i + 0.5) / len(seq), ti, i, o))
                    items.sort(key=lambda x: (x[0], x[1], x[2]))
                    glob.extend(x[3] for x in items)
                per = {e: [o for o in glob if o.eng == e] for e in self.ENGS}
            for e in self.ENGS:
                for op in per[e]:
                    if op.dma:
                        i = qn[op.eng]
                        qn[op.eng] += 1
                        s = qsem[op.eng][i % self.KDMA]
                        op.sig = (s, 16 * (i // self.KDMA + 1))
                        if i >= self.KDMA:
                            op.thr = (s, 16 * (i // self.KDMA))
                    elif op.needed:
                        ecount[op.eng] += 1
                        op.sig = (esem[op.eng], ecount[op.eng])
            block = es.enter_context(nc.Block())

            def run(ename, eng):
                waited = {}

                def emit_op(op, waited):
                    ws = [d.sig for d in op.deps]
                    if op.thr is not None:
                        ws.append(op.thr)
                    for (s, v) in ws:
                        k = id(s)
                        if waited.get(k, 0) >= v:
                            continue
                        waited[k] = v
                        eng.wait_ge(s, v)
                    ins = op.fn(eng)
                    if op.sig is not None:
                        ins.then_inc(op.sig[0], 16 if op.dma else 1)

                ops = per[ename]
                i = 0
                creg = None
                curkey = None
                while i < len(ops):
                    c = ops[i].cond
                    j = i
                    while j < len(ops) and ops[j].cond == c:
                        j += 1
                    seg = ops[i:j]
                    i = j
                    if c is None:
                        for op in seg:
                            emit_op(op, waited)
                        continue
                    key, thr = c[0], c[1]
                    hi = c[2] if len(c) > 2 else None
                    if creg is None:
                        creg = eng.alloc_register("creg_" + u + ename)
                        d = self.cnt_dep
                        if d is not None and waited.get(id(d.sig[0]), 0) < d.sig[1]:
                            waited[id(d.sig[0])] = d.sig[1]
                            eng.wait_ge(d.sig[0], d.sig[1])
                    if curkey != key:
                        eng.reg_load(creg, self.cnt_ap(key))
                        curkey = key
                    saved = dict(waited)

                    def comp():
                        w2 = dict(saved)
                        ncomp = sum(1 for op in seg if (not op.dma) and op.sig is not None)
                        if ncomp:
                            eng.drain()
                            eng.sem_inc(esem[ename], ncomp)
                        for op in seg:
                            if op.dma:
                                if op.thr is not None and w2.get(id(op.thr[0]), 0) < op.thr[1]:
                                    w2[id(op.thr[0])] = op.thr[1]
                                    eng.wait_ge(op.thr[0], op.thr[1])
                                eng.sem_inc(op.sig[0], 16)

                    def body():
                        w3 = dict(saved)
                        for op in seg:
                            emit_op(op, w3)

                    with eng.If_lt(creg, thr + 1):
                        comp()
                    with eng.Else():
                        if hi is None:
                            body()
                        else:
                            with eng.If_lt(creg, hi + 1):
                                body()
                            with eng.Else():
                                comp()
                    waited = saved
                if ename in qsem:
                    n = qn[ename]
                    for i2, s in enumerate(qsem[ename]):
                        cnt = (n - i2 + self.KDMA - 1) // self.KDMA if n > i2 else 0
                        if cnt > 0 and waited.get(id(s), 0) < 16 * cnt:
                            eng.wait_ge(s, 16 * cnt)

            @block.sync
            def _(e):
                run("sp", e)

            @block.gpsimd
            def _(e):
                run("pool", e)

            @block.scalar
            def _(e):
                run("act", e)

            @block.vector
            def _(e):
                run("dve", e)

            @block.tensor
            def _(e):
                run("pe", e)

import os
import numpy as np
from contextlib import ExitStack
from concourse.bass_utils import run_bass_kernel_spmd

NBC = 2
SEQ = 2048
NCTX = 256
TOK = SEQ + NCTX
NT = TOK // 128
D = 1024
NE = 32
CAP = 1536
NSLOT = NE * CAP
EPS = 1e-6
BIG = 1.0e6
NOPOOL = True
PIPE = True
NOACTCP = True


_U = [0]


class Dbl:
    def __init__(self, p, a, b):
        self.p = p
        self.t = (a, b)

    def __getitem__(self, k):
        return self.t[self.p.par][k]


class Ph:
    def __init__(self, nc):
        _U[0] += 1
        self.nc = nc
        self.par = 0
        self.nopool = NOPOOL
        self.sc = Sched(nc)

    def setpar(self, it):
        self.par = it % 2
        self.sc.sfx = "#%d" % (it % 2)

    def reg(self, e, val):
        if not hasattr(self, "_regs"):
            self._regs = {}
        if val not in self._regs:
            self._regs[val] = e.to_reg(val)
        return self._regs[val]

    def mm(self, out, lhsT, rhs, start, stop, r, w):
        self.sc.op("pe", lambda e: e.matmul(out, lhsT=lhsT, rhs=rhs, start=start, stop=stop), r, w)

    def tr(self, out, in_, ident, r, w):
        self.sc.op("pe", lambda e: e.transpose(out=out, in_=in_, identity=ident), r, w)

    def act(self, out, in_, func, r, w, scale=1.0, bias=None, accum=None):
        kw = {}
        if NOACTCP and func == AF.Copy and accum is None and bias is None:
            self.sc.op("dve", lambda e: e.tensor_scalar(out=out, in0=in_, scalar1=float(scale), scalar2=None, op0=ALU.mult), r, w)
            return
        if accum is not None:
            kw["accum_out"] = accum
        if bias is not None:
            kw["bias"] = bias
        self.sc.op("act", lambda e: e.activation(out=out, in_=in_, func=func, scale=scale, **kw), r, w)

    def tt(self, eng, out, a, b, op, r, w):
        eng = "dve" if (eng == "pool" and self.nopool) else eng
        self.sc.op(eng, lambda e: e.tensor_tensor(out=out, in0=a, in1=b, op=op), r, w)

    def ts(self, eng, out, a, s1, s2, op0, op1, r, w):
        eng = "dve" if (eng == "pool" and self.nopool) else eng
        if s2 is None:
            self.sc.op(eng, lambda e: e.tensor_scalar(out=out, in0=a, scalar1=s1, scalar2=None, op0=op0), r, w)
        else:
            self.sc.op(eng, lambda e: e.tensor_scalar(out=out, in0=a, scalar1=s1, scalar2=s2, op0=op0, op1=op1), r, w)

    def stt(self, out, in0, scalar, in1, op0, op1, r, w, accum=None):
        kw = {}
        if accum is not None:
            kw["accum_out"] = accum
        self.sc.op("dve", lambda e: e.scalar_tensor_tensor(out=out, in0=in0, scalar=scalar, in1=in1, op0=op0, op1=op1, **kw), r, w)

    def cp(self, eng, out, in_, r, w):
        eng = "dve" if (eng == "pool" and self.nopool) else eng
        if eng == "act" and NOACTCP:
            eng = "dve"
        if eng == "act":
            self.sc.op(eng, lambda e: e.copy(out=out, in_=in_), r, w)
        else:
            self.sc.op(eng, lambda e: e.tensor_copy(out=out, in_=in_), r, w)

    def rcp(self, out, in_, r, w):
        self.sc.op("dve", lambda e: e.reciprocal(out=out, in_=in_), r, w)

    def dma(self, q, out, in_, r, w):
        self.sc.dma(q, lambda e: e.dma_start(out=out, in_=in_), r, w)

    def memset(self, eng, ap, val, w):
        eng = "dve" if (eng == "pool" and self.nopool) else eng
        self.sc.op(eng, lambda e: e.memset(ap, val), (), w)

    def rstd(self, ss, n, name, dim):
        self.act(ss[:, 16:17], ss[:, 0:1], AF.Sqrt, [name], [name + "_s"], scale=1.0 / dim, bias=self.eps_ap)
        self.rcp(ss[:, 32:33], ss[:, 16:17], [name + "_s"], [name + "_r"])


def _consts(p, es, nc, T):
    sb = lambda name, shape, dt: es.enter_context(nc.sbuf_tensor("u%d_" % _U[0] + name, shape, dt))
    identf = sb("identf", [128, 128], F32)
    identb = sb("identb", [128, 128], BF16)
    epst = sb("epst", [128, 1], F32)
    p.dma("sp", identf[:], T["IDENT"], [], ["identf"])
    p.cp("dve", identb[:], identf[:], ["identf"], ["identb"])
    p.memset("pool", epst[:], EPS, ["eps"])
    p.eps_ap = epst[:, 0:1]
    return identf, identb


def phase_mod(nc, T):
    with nc.cleanup_on_exit(), ExitStack() as es:
        sb = lambda name, shape, dt: es.enter_context(nc.sbuf_tensor("u%d_" % _U[0] + name, shape, dt))
        pm = lambda name, shape, dt: es.enter_context(nc.psum_tensor("u%d_" % _U[0] + name, shape, dt))
        p = Ph(nc)
        cT = sb("cT", [128, 3, 8], F32)
        wb = [sb("wb%d" % i, [128, 8, 512], F32) for i in range(2)]
        adab = sb("adab", [3, 6144], F32)
        ng = sb("ng", [3, 2, 1024], F32)
        mod = sb("mod", [3, 6144], F32)
        tmp = sb("tmp", [3, 512], F32)
        ps = [pm("psm%d" % i, [128, 512], F32) for i in range(2)]
        for j in range(2):
            p.dma("sp", cT[:, j, :], T["c"][j].rearrange("(p k) -> p k", k=8), [], ["cT"])
        p.dma("sp", cT[:, 2, :], T["c_ctx"].rearrange("(p k) -> p k", k=8), [], ["cT"])
        p.act(cT[:], cT[:], AF.Silu, ["cT"], ["cT"])
        it = 0
        for i in range(2):
            p.dma("sp", adab[:], T["ada_b"][i].partition_broadcast(3), [], ["adab"])
            p.dma("sp", ng[:, 0, :], T["norm_mix_g"][i].partition_broadcast(3), [], ["ng"])
            p.dma("sp", ng[:, 1, :], T["norm_ffn_g"][i].partition_broadcast(3), [], ["ng"])
            wv = T["ada_w"][i].rearrange("(p k) n -> p k n", k=8)
            for n in range(12):
                w_ = wb[it % 2]
                wn = "wb%d" % (it % 2)
                pn = "ps%d" % (it % 2)
                pp = ps[it % 2]
                it += 1
                p.dma("sp", w_[:], wv[:, :, n * 512:(n + 1) * 512], [], [wn])
                for k in range(8):
                    p.mm(pp[0:3, :], cT[:, :, k], w_[:, k, :], k == 0, k == 7, ["cT", wn], [pn])
                sl = slice(n * 512, (n + 1) * 512)
                if n in (2, 3, 8, 9):
                    gi = 0 if n < 4 else 1
                    go = (n % 2) * 512
                    p.tt("dve", tmp[:], pp[0:3, :], adab[:, sl], ALU.add, [pn, "adab"], ["tmp"])
                    p.stt(mod[:, sl], tmp[:], 1.0, ng[:, gi, go:go + 512], ALU.add, ALU.mult, ["tmp", "ng"], ["mod"])
                else:
                    p.tt("dve", mod[:, sl], pp[0:3, :], adab[:, sl], ALU.add, [pn, "adab"], ["mod"])
            p.dma("sp", T["MOD"][i], mod[:], ["mod"], ["MODd"])
        p.sc.emit()


def _load_mod(p, tile, T, layer, idx, name):
    for j in range(3):
        p.dma("sp", tile[:, j, :], T["MOD"][layer, j, idx * 1024:(idx + 1) * 1024].partition_broadcast(128), [], [name])


def _rope(p, x1, x2, cos, sin, o1, o2, tmps, rd, wr1, wr2):
    ra, rb, rc, rd_ = tmps
    p.tt("dve", ra, x1, cos, ALU.mult, rd, ["ra"])
    p.tt("pool", rb, x2, sin, ALU.mult, rd, ["rb"])
    p.tt("dve", o1, ra, rb, ALU.subtract, ["ra", "rb"], [wr1])
    p.tt("pool", rc, x2, cos, ALU.mult, rd, ["rc"])
    p.tt("dve", rd_, x1, sin, ALU.mult, rd, ["rd"])
    p.tt("pool", o2, rc, rd_, ALU.add, ["rc", "rd"], [wr2])


def phase_p1(nc, T, layer):
    with nc.cleanup_on_exit(), ExitStack() as es:
        sb = lambda name, shape, dt: es.enter_context(nc.sbuf_tensor("u%d_" % _U[0] + name, shape, dt))
        pm = lambda name, shape, dt: es.enter_context(nc.psum_tensor("u%d_" % _U[0] + name, shape, dt))
        p = Ph(nc)
        dsb = lambda name, shape, dt: Dbl(p, sb(name + "A", shape, dt), sb(name + "B", shape, dt))
        p.sc.dbl = {"junk", "ss", "ss_s", "ss_r", "t1", "hb", "hT", "qk_q0", "qk_q1", "qk_k", "va", "qkr1", "qkr2", "ra", "rb", "rc", "rd",
                    "qkT", "ssq", "ssq_s", "ssq_r", "ssk", "ssk_s", "ssk_r", "cn_q", "cn_k", "kr", "krr1", "krr2", "cT", "qb_a", "qb_b",
                    "qb_r1", "qb_r2", "qr_a", "qr_b", "kb_n0", "kb_n1", "kb_r", "vb0", "vb1", "qbT", "kbT", "qk0", "qk1", "qk2", "vc",
                    "sq", "sq2", "ssh", "ssh_s", "ssh_r", "qkn", "qT_q", "qT_k"}
        identf, identb = _consts(p, es, nc, T)
        ncol = 1440 if layer == 0 else 1536
        win = sb("win", [128, 8, ncol], BF16)
        G1 = sb("G1", [128, 3, 1024], F32)
        S1 = sb("S1", [128, 3, 1024], F32)
        xt = [sb("xt%d" % i, [128, 1024], F32) for i in range(2)]
        rp = [sb("rp%d" % i, [128, 128], F32) for i in range(3)]
        junk = dsb("junk", [128, 1024], BF16)
        ss = dsb("ss", [128, 48], F32)
        t1 = dsb("t1", [128, 1024], F32)
        hb = dsb("hb", [128, 1024], BF16)
        hT = dsb("hT", [128, 8, 128], BF16)
        ptr = pm("ptr", [128, 8, 128], BF16)
        pp = [pm("pp%d" % i, [128, 512], F32) for i in range(4)]
        ptq = pm("ptq", [128, 8, 128], BF16)
        ptk = pm("ptk", [128, 8, 128], BF16)
        wname = "ab_w_in" if layer == 0 else "c_w_in"
        p.sc.dma("pool", lambda e: e.dma_start(out=win[:], in_=T[wname].rearrange("(k p) n -> p k n", p=128)), (), ["win"])
        _load_mod(p, S1, T, layer, 0, "S1")
        _load_mod(p, G1, T, layer, 1, "G1")
        if layer == 0:
            wqb = sb("wqb", [128, 3, 768], BF16)
            wkvb = sb("wkvb", [128, 2, 1024], BF16)
            gq = sb("gq", [128, 384], F32)
            gkv = sb("gkv", [128, 256], F32)
            p.sc.dma("pool", lambda e: e.dma_start(out=wqb[:], in_=T["mla_wq_b"].rearrange("(k p) n -> p k n", p=128)), (), ["wqb"])
            p.sc.dma("pool", lambda e: e.dma_start(out=wkvb[:], in_=T["mla_wkv_b"].rearrange("(k p) n -> p k n", p=128)), (), ["wkvb"])
            p.dma("sp", gq[:], T["mla_q_norm_g"].partition_broadcast(128), [], ["gq"])
            p.dma("sp", gkv[:], T["mla_kv_norm_g"].partition_broadcast(128), [], ["gkv"])
            qk = dsb("qk", [128, 10, 64], F32)
            qkr = dsb("qkr", [128, 10, 64], BF16)
            rtm = [dsb("rtm%d" % i, [128, 10, 32], F32) for i in range(4)]
            va = dsb("va", [128, 2, 65], BF16)
            qkT = dsb("qkT", [128, 5, 128], BF16)
            ssq = dsb("ssq", [128, 48], F32)
            ssk = dsb("ssk", [128, 48], F32)
            cn = dsb("cn", [128, 640], BF16)
            kr = dsb("kr", [128, 32], F32)
            krr = dsb("krr", [128, 32], BF16)
            cT = dsb("cT", [128, 5, 128], BF16)
            qb = dsb("qb", [128, 8, 96], BF16)
            qr = dsb("qr", [128, 8, 32], F32)
            kb = dsb("kb", [128, 8, 96], BF16)
            vb = dsb("vb", [128, 8, 65], BF16)
            qbT = dsb("qbT", [96, 8, 128], BF16)
            kbT = dsb("kbT", [96, 8, 128], BF16)
            for _i in range(2):
                p.setpar(_i)
                p.memset("pool", va[:], 1.0, ["va"])
                p.memset("pool", vb[:], 1.0, ["vb0", "vb1"])
            p.setpar(0)
            zt = sb("zt", [128, 4096], BF16)
            p.memset("dve", zt[:], 0.0, ["zt"])
            xsv = T["XS"].rearrange("(c p r) d -> c p (r d)", p=128, r=4)
            for c in range(NSLOT // 512):
                p.sc.dma("pool", lambda e_, c=c: e_.dma_start(out=xsv[c], in_=zt[:]), ["zt"], ["XSz%d" % (c % 4)])
        else:
            gqk = sb("gqk", [128, 10, 128], F32)
            gtmp = sb("gtmp", [128, 2, 128], F32)
            p.dma("sp", gtmp[:, 0, :], T["c_q_norm_g"].partition_broadcast(128), [], ["gtmp"])
            p.dma("sp", gtmp[:, 1, :], T["c_k_norm_g"].partition_broadcast(128), [], ["gtmp"])
            p.act(gqk[:, 0:8, :], gtmp[:, 0:1, :].to_broadcast([128, 8, 128]), AF.Copy, ["gtmp"], ["gqk"], scale=128.0 ** -0.5)
            p.cp("dve", gqk[:, 8:10, :], gtmp[:, 1:2, :].to_broadcast([128, 2, 128]), ["gtmp"], ["gqk"])
            qk = dsb("qk", [128, 10, 128], F32)
            sq = dsb("sq", [128, 10, 128], F32)
            ssh = dsb("ssh", [128, 30], F32)
            qkn = dsb("qkn", [128, 10, 128], F32)
            qkr = dsb("qkr", [128, 10, 128], BF16)
            rtm = [dsb("rtm%d" % i, [128, 10, 64], F32) for i in range(4)]
            vc = dsb("vc", [128, 2, 129], BF16)
            qT = dsb("qT", [128, 10, 128], BF16)
            for _i in range(2):
                p.setpar(_i)
                p.memset("pool", vc[:], 1.0, ["vc"])
            p.setpar(0)

        tiles = [(b, t) for b in range(NBC) for t in range(NT)]

        def src_of(b, t):
            if layer == 0:
                if t < 16:
                    return T["x"][b, t * 128:(t + 1) * 128, :]
                return T["ctx"][b, (t - 16) * 128:(t - 15) * 128, :]
            return T["XR"][b, t * 128:(t + 1) * 128, :]

        rope_t = T["ROPE0"] if layer == 0 else T["ROPE1"]
        rw = 96 if layer == 0 else 128

        def load(it):
            b, t = tiles[it]
            p.dma("sp", xt[it % 2][:], src_of(b, t), [], ["xt%d" % (it % 2)])
            if t < 16:
                p.dma("sp", rp[it % 3][:, 0:rw], rope_t[t * 128:(t + 1) * 128, :], [], ["rp%d" % (it % 3)])

        load(0)
        for it, (b, t) in enumerate(tiles):
            if PIPE and layer == 1:
                p.sc.key = (it, 0)
            if it + 1 < len(tiles):
                load(it + 1)
            p.setpar(it)
            lat = t < 16
            j = b if lat else 2
            x = xt[it % 2]
            xn = "xt%d" % (it % 2)
            r_ = rp[it % 3]
            rn = "rp%d" % (it % 3)
            rows = slice(t * 128, (t + 1) * 128)
            p.act(junk[:], x[:], AF.Square, [xn], ["junk", "ss"], accum=ss[:, 0:1])
            p.rstd(ss, 1, "ss", 1024)
            p.stt(t1[:], x[:], ss[:, 32:33], G1[:, j, :], ALU.mult, ALU.mult, [xn, "ss_r", "G1"], ["t1"])
            p.tt("pool", hb[:], t1[:], S1[:, j, :], ALU.add, ["t1", "S1"], ["hb"])
            for k in range(8):
                p.tr(ptr[:, k, :], hb[:, k * 128:(k + 1) * 128], identb[:], ["hb", "identb"], ["ptr"])
            p.cp("act", hT[:], ptr[:], ["ptr"], ["hT"])
            if layer == 0:
                chunks = [(0, 512), (512, 768), (768, 1152), (1152, 1440)]
                for ci, (c0, c1) in enumerate(chunks):
                    for k in range(8):
                        p.mm(pp[ci][:, 0:c1 - c0], hT[:, k, :], win[:, k, c0:c1], k == 0, k == 7, ["hT", "win"], ["pp%d" % ci])
                for g in range(2):
                    p.act(qk[:, 0:8, :].rearrange("p (j g) d -> p j g d", g=2)[:, :, g, :],
                          pp[0][:, g * 256:(g + 1) * 256].rearrange("p (j d) -> p j d", j=4), AF.Copy, ["pp0"], ["qk_q%d" % g], scale=0.125)
                p.cp("dve", qk[:, 8:10, :], pp[1][:, 0:128].rearrange("p (h d) -> p h d", h=2), ["pp1"], ["qk_k"])
                p.cp("dve", va[:, :, 0:64], pp[1][:, 128:256].rearrange("p (h d) -> p h d", h=2), ["pp1"], ["va"])
                if lat:
                    bc = lambda a: a.unsqueeze(1).to_broadcast([128, 10, 32])
                    _rope(p, qk[:, :, 0:32], qk[:, :, 32:64], bc(r_[:, 0:32]), bc(r_[:, 32:64]),
                          qkr[:, :, 0:32], qkr[:, :, 32:64], [m[:] for m in rtm], ["qk_q0", "qk_q1", "qk_k", rn], "qkr1", "qkr2")
                else:
                    p.cp("dve", qkr[:], qk[:], ["qk_q0", "qk_q1", "qk_k"], ["qkr1", "qkr2"])
                qkr2d = qkr[:].rearrange("p h d -> p (h d)")
                for c in range(5):
                    p.tr(ptr[:, c, :], qkr2d[:, c * 128:(c + 1) * 128], identb[:], ["qkr1", "qkr2", "identb"], ["ptr"])
                p.cp("act", qkT[:], ptr[:, 0:5, :], ["ptr"], ["qkT"])
                p.dma("sp", T["QAT"][b][:, :, rows], qkT[:, 0:4, :], ["qkT"], ["QAT"])
                p.dma("sp", T["KAT"][b][:, rows], qkT[:, 4, :], ["qkT"], ["KAT"])
                p.dma("sp", T["VA1"][b][rows], va[:], ["va"], ["VA1"])
                p.act(junk[:, 0:384], pp[2][:, 0:384], AF.Square, ["pp2"], ["junk", "ssq"], accum=ssq[:, 0:1])
                p.rstd(ssq, 1, "ssq", 384)
                p.act(junk[:, 0:256], pp[3][:, 0:256], AF.Square, ["pp3"], ["junk", "ssk"], accum=ssk[:, 0:1])
                p.rstd(ssk, 1, "ssk", 256)
                p.stt(cn[:, 0:384], pp[2][:, 0:384], ssq[:, 32:33], gq[:], ALU.mult, ALU.mult, ["pp2", "ssq_r", "gq"], ["cn_q"])
                p.stt(cn[:, 384:640], pp[3][:, 0:256], ssk[:, 32:33], gkv[:], ALU.mult, ALU.mult, ["pp3", "ssk_r", "gkv"], ["cn_k"])
                p.cp("act", kr[:], pp[3][:, 256:288], ["pp3"], ["kr"])
                if lat:
                    _rope(p, kr[:, 0:16], kr[:, 16:32], r_[:, 64:80], r_[:, 80:96], krr[:, 0:16], krr[:, 16:32],
                          [m[:, 0, 0:16] for m in rtm], ["kr", rn], "krr1", "krr2")
                else:
                    p.cp("dve", krr[:], kr[:], ["kr"], ["krr1", "krr2"])
                for c in range(5):
                    p.tr(ptr[:, c, :], cn[:, c * 128:(c + 1) * 128], identb[:], ["cn_q", "cn_k", "identb"], ["ptr"])
                p.cp("act", cT[:], ptr[:, 0:5, :], ["ptr"], ["cT"])
                for ci, (c0, c1) in enumerate([(0, 480), (480, 768)]):
                    for k in range(3):
                        p.mm(pp[ci][:, 0:c1 - c0], cT[:, k, :], wqb[:, k, c0:c1], k == 0, k == 2, ["cT", "wqb"], ["pp%d" % ci])
                for ci in range(2):
                    for k in range(2):
                        p.mm(pp[2 + ci][:, 0:512], cT[:, 3 + k, :], wkvb[:, k, ci * 512:(ci + 1) * 512], k == 0, k == 1, ["cT", "wkvb"], ["pp%d" % (2 + ci)])
                s = 96.0 ** -0.5
                qv0 = pp[0][:, 0:480].rearrange("p (h d) -> p h d", h=5)
                qv1 = pp[1][:, 0:288].rearrange("p (h d) -> p h d", h=3)
                if lat:
                    p.act(qb[:, 0:5, 0:64], qv0[:, :, 0:64], AF.Copy, ["pp0"], ["qb_a"], scale=s)
                    p.act(qb[:, 5:8, 0:64], qv1[:, :, 0:64], AF.Copy, ["pp1"], ["qb_b"], scale=s)
                    p.act(qr[:, 0:5, :], qv0[:, :, 64:96], AF.Copy, ["pp0"], ["qr_a"], scale=s)
                    p.act(qr[:, 5:8, :], qv1[:, :, 64:96], AF.Copy, ["pp1"], ["qr_b"], scale=s)
                    bc8 = lambda a: a.unsqueeze(1).to_broadcast([128, 8, 16])
                    _rope(p, qr[:, :, 0:16], qr[:, :, 16:32], bc8(r_[:, 64:80]), bc8(r_[:, 80:96]),
                          qb[:, :, 64:80], qb[:, :, 80:96], [m[:, 0:8, 0:16] for m in rtm], ["qr_a", "qr_b", rn], "qb_r1", "qb_r2")
                else:
                    p.act(qb[:, 0:5, :], qv0, AF.Copy, ["pp0"], ["qb_a", "qb_r1"], scale=s)
                    p.act(qb[:, 5:8, :], qv1, AF.Copy, ["pp1"], ["qb_b", "qb_r2"], scale=s)
                for ci in range(2):
                    kvv = pp[2 + ci][:, 0:512].rearrange("p (h d) -> p h d", h=4)
                    p.cp("dve", kb[:, 4 * ci:4 * ci + 4, 0:64], kvv[:, :, 0:64], ["pp%d" % (2 + ci)], ["kb_n%d" % ci])
                    p.cp("act", vb[:, 4 * ci:4 * ci + 4, 0:64], kvv[:, :, 64:128], ["pp%d" % (2 + ci)], ["vb%d" % ci])
                p.cp("pool", kb[:, :, 64:96], krr[:].unsqueeze(1).to_broadcast([128, 8, 32]), ["krr1", "krr2"], ["kb_r"])
                for h in range(8):
                    p.tr(ptq[0:96, h, :], qb[:, h, :], identb[:], ["qb_a", "qb_b", "qb_r1", "qb_r2", "identb"], ["ptq"])
                for h in range(8):
                    p.tr(ptk[0:96, h, :], kb[:, h, :], identb[:], ["kb_n0", "kb_n1", "kb_r", "identb"], ["ptk"])
                p.cp("dve", qbT[:], ptq[0:96], ["ptq"], ["qbT"])
                p.cp("act", kbT[:], ptk[0:96], ["ptk"], ["kbT"])
                p.dma("sp", T["QBT"][b][:, :, rows], qbT[:], ["qbT"], ["QBT"])
                p.dma("sp", T["KBT"][b][:, :, rows], kbT[:], ["kbT"], ["KBT"])
                p.dma("sp", T["VB1"][b][rows], vb[:], ["vb0", "vb1"], ["VB1"])
            else:
                h0 = 0 if lat else 8
                if lat:
                    for ci in range(2):
                        for k in range(8):
                            p.mm(pp[ci][:], hT[:, k, :], win[:, k, ci * 512:(ci + 1) * 512], k == 0, k == 7, ["hT", "win"], ["pp%d" % ci])
                for k in range(8):
                    p.mm(pp[2][:], hT[:, k, :], win[:, k, 1024:1536], k == 0, k == 7, ["hT", "win"], ["pp2"])
                if lat:
                    p.cp("act", qk[:, 0:4, :], pp[0][:].rearrange("p (h d) -> p h d", h=4), ["pp0"], ["qk0"])
                    p.cp("act", qk[:, 4:8, :], pp[1][:].rearrange("p (h d) -> p h d", h=4), ["pp1"], ["qk1"])
                p.cp("act", qk[:, 8:10, :], pp[2][:, 0:256].rearrange("p (h d) -> p h d", h=2), ["pp2"], ["qk2"])
                p.cp("dve", vc[:, :, 0:128], pp[2][:, 256:512].rearrange("p (h d) -> p h d", h=2), ["pp2"], ["vc"])
                if PIPE:
                    p.sc.key = (it, 1)
                qkd = ["qk0", "qk1", "qk2"]
                nh = 10 - h0
                p.tt("pool", sq[:, h0:10, :], qk[:, h0:10, :], qk[:, h0:10, :], ALU.mult, qkd, ["sq"])
                p.sc.op("dve", lambda e, o_=ssh[:, h0:10], i_=sq[:, h0:10, :]: e.tensor_reduce(out=o_, in_=i_, axis=AX.X, op=ALU.add), ["sq"], ["ssh"])
                p.act(ssh[:, 10 + h0:20], ssh[:, h0:10], AF.Sqrt, ["ssh"], ["ssh_s"], scale=1.0 / 128, bias=p.eps_ap)
                p.rcp(ssh[:, 20 + h0:30], ssh[:, 10 + h0:20], ["ssh_s"], ["ssh_r"])
                p.tt("dve", sq[:, h0:10, :], qk[:, h0:10, :], ssh[:, 20 + h0:30].unsqueeze(2).to_broadcast([128, nh, 128]), ALU.mult, qkd + ["ssh_r", "sq"], ["sq2"])
                p.tt("pool", qkn[:, h0:10, :], sq[:, h0:10, :], gqk[:, h0:10, :], ALU.mult, ["sq2", "gqk"], ["qkn"])
                if lat:
                    bc = lambda a: a.unsqueeze(1).to_broadcast([128, 10, 64])
                    _rope(p, qkn[:, :, 0:64], qkn[:, :, 64:128], bc(r_[:, 0:64]), bc(r_[:, 64:128]),
                          qkr[:, :, 0:64], qkr[:, :, 64:128], [m[:] for m in rtm], ["qkn", rn], "qkr1", "qkr2")
                else:
                    p.cp("dve", qkr[:, 8:10, :], qkn[:, 8:10, :], ["qkn"], ["qkr1", "qkr2"])
                if lat:
                    for h in range(8):
                        p.tr(ptq[:, h, :], qkr[:, h, :], identb[:], ["qkr1", "qkr2", "identb"], ["ptq"])
                    p.cp("act", qT[:, 0:8, :], ptq[:], ["ptq"], ["qT_q"])
                    p.dma("sp", T["QCT"][b][:, :, rows], qT[:, 0:8, :], ["qT_q"], ["QCT"])
                for h in range(2):
                    p.tr(ptk[:, h, :], qkr[:, 8 + h, :], identb[:], ["qkr1", "qkr2", "identb"], ["ptk"])
                p.cp("dve", qT[:, 8:10, :], ptk[:, 0:2, :], ["ptk"], ["qT_k"])
                p.dma("sp", T["KCT"][b][:, :, rows], qT[:, 8:10, :], ["qT_k"], ["KCT"])
                p.dma("sp", T["VC1"][b][rows], vc[:], ["vc"], ["VC1"])
        p.sc.key = None
        p.sc.emit()


def phase_attn_a(nc, T):
    with nc.cleanup_on_exit(), ExitStack() as es:
        sb = lambda name, shape, dt: es.enter_context(nc.sbuf_tensor("u%d_" % _U[0] + name, shape, dt))
        pm = lambda name, shape, dt: es.enter_context(nc.psum_tensor("u%d_" % _U[0] + name, shape, dt))
        p = Ph(nc)
        qat = [sb("qat%d" % i, [128, 4, TOK], BF16) for i in range(2)]
        kat = [sb("kat%d" % i, [128, TOK], BF16) for i in range(2)]
        va1 = [sb("va1%d" % i, [128, NT, 2, 65], BF16) for i in range(2)]
        esink = sb("esink", [128, 8], F32)
        mk = sb("mk", [128, 2, 128], F32)
        mkb = sb("mkb", [128, 2, 128], BF16)
        pT = [sb("pT%d" % i, [128, 5, 512], BF16) for i in range(2)]
        den = sb("den", [128, 16], F32)
        osb = [sb("osb%d" % i, [128, 4, 64], BF16) for i in range(2)]
        psc = [pm("psc%d" % i, [128, 512], F32) for i in range(4)]
        po = [[pm("po%d_%d" % (i, h), [128, 2, 65], F32) for h in range(2)] for i in range(2)]
        p.dma("sp", esink[:], T["swa_sink"].partition_broadcast(128), [], ["esink"])
        p.act(esink[:], esink[:], AF.Exp, ["esink"], ["esink"])
        p.dma("sp", mk[:], T["MASKS"].rearrange("m p q -> p m q"), [], ["mk"])
        p.cp("dve", mkb[:], mk[:], ["mk"], ["masks"])
        zf = sb("zf", [128, 1024], F32)
        p.memset("pool", zf[:], 0.0, ["zf"])
        p.dma("sp", T["YS"][NSLOT:NSLOT + 128, :], zf[:], ["zf"], ["YSz"])
        cnt = [0, 0]
        for b in range(NBC):
            p.dma("sp", qat[b][:], T["QAT"][b], [], ["qat%d" % b])
            p.dma("sp", kat[b][:], T["KAT"][b], [], ["kat%d" % b])
            p.dma("sp", va1[b][:], T["VA1"][b].rearrange("(t p) g d -> p t g d", p=128), [], ["va1%d" % b])
        pend = None
        for b in range(NBC):
            for qi in range(NT):
                for g in range(2):
                    if qi < 16:
                        lk = [k for k in (qi - 1, qi, qi + 1) if 0 <= k < 16]
                        kts = lk + [16, 17]
                        masks = [mkb[:, 0, :] if k == qi - 1 else (mkb[:, 1, :] if k == qi + 1 else None) for k in lk] + [None, None]
                    else:
                        kts = [16, 17]
                        masks = None
                    o_ = osb[cnt[0] % 2]
                    on = "osb%d" % (cnt[0] % 2)
                    pvf = _attn_core_named(p, kat[b][g * 64:(g + 1) * 64, :], "kat%d" % b,
                                           qat[b][g * 64:(g + 1) * 64, :, qi * 128:(qi + 1) * 128], "qat%d" % b,
                                           va1[b], "va1%d" % b, g, kts, 512, 64, masks, psc, pT, po, den, o_, on,
                                           esink[:, 4 * g:4 * g + 4], cnt)
                    if pend is not None:
                        pend()

                    def pend(pvf=pvf, b=b, qi=qi, g=g, o_=o_, on=on):
                        pvf()
                        p.dma("sp", T["OATT"][b][qi * 128:(qi + 1) * 128, g * 256:(g + 1) * 256],
                              o_[:].rearrange("p j d -> p (j d)"), [on + "_0", on + "_1"], ["OATT"])
        pend()
        p.sc.emit()


def _attn_core_named(p, kT, kname, qv, qname, v1, vname, hv, kts, nq, dv, masks, psc, pT, po, den, osb, osbn, sink_ap, cnt):
    nk = len(kts)
    nj = nq // 128
    base = cnt[0]
    cnt[0] += 1
    pTt = pT[base % 2]
    pTn = "pT%d" % (base % 2)
    for n, kt in enumerate(kts):
        ps = psc[cnt[1] % 4]
        psn = "psc%d" % (cnt[1] % 4)
        cnt[1] += 1
        p.mm(ps[:, 0:nq], kT[:, kt * 128:(kt + 1) * 128], qv, True, True, [kname, qname], [psn])
        p.act(pTt[:, n, 0:nq], ps[:, 0:nq], AF.Exp, [psn], [pTn + "_%d" % n])
        if masks is not None and masks[n] is not None:
            v_ = pTt[:, n, 0:nq].rearrange("p (j q) -> p j q", q=128)
            p.tt("pool", v_, v_, masks[n].unsqueeze(1).to_broadcast([128, nj, 128]), ALU.mult, [pTn + "_%d" % n, "masks"], [pTn + "_%d" % n])
    return lambda: _attn_pv(p, base, pTt, pTn, v1, vname, hv, kts, nj, dv, po, den, osb, osbn, sink_ap)


def _attn_pv(p, base, pTt, pTn, v1, vname, hv, kts, nj, dv, po, den, osb, osbn, sink_ap):
    nk = len(kts)
    pset = po[base % 2]
    pon = "po%d" % (base % 2)
    dnb = "den%d" % (base % 2)
    dd = den[:, (base % 2) * 8:(base % 2) * 8 + 8]
    for j in range(nj):
        pj = pset[j // 2][:, j % 2, 0:dv + 1]
        for n, kt in enumerate(kts):
            p.mm(pj, pTt[:, n, j * 128:(j + 1) * 128], v1[:, kt, hv, 0:dv + 1], n == 0, n == nk - 1,
                 [pTn + "_%d" % n, vname], [pon + "_%d" % (j // 2)])
    for half in range((nj + 1) // 2):
        j0 = half * 2
        nn = min(2, nj - j0)
        pv = pset[half]
        dn = dnb + "_%d" % half
        if sink_ap is not None:
            p.tt("dve", dd[:, j0:j0 + nn], pv[:, 0:nn, dv], sink_ap[:, j0:j0 + nn], ALU.add, [pon + "_%d" % half, "esink"], [dn])
            p.rcp(dd[:, 4 + j0:4 + j0 + nn], dd[:, j0:j0 + nn], [dn], [dn + "r"])
        else:
            p.rcp(dd[:, 4 + j0:4 + j0 + nn], pv[:, 0:nn, dv], [pon + "_%d" % half], [dn + "r"])
        p.tt("dve", osb[:, j0:j0 + nn, 0:dv], pv[:, 0:nn, 0:dv], dd[:, 4 + j0:4 + j0 + nn].unsqueeze(2).to_broadcast([128, nn, dv]),
             ALU.mult, [pon + "_%d" % half, dn + "r"], [osbn + "_%d" % half])


def phase_attn_dense(nc, T, layer):
    with nc.cleanup_on_exit(), ExitStack() as es:
        sb = lambda name, shape, dt: es.enter_context(nc.sbuf_tensor("u%d_" % _U[0] + name, shape, dt))
        pm = lambda name, shape, dt: es.enter_context(nc.psum_tensor("u%d_" % _U[0] + name, shape, dt))
        p = Ph(nc)
        if layer == 0:
            hd, dv, nkv, nqc = 96, 64, 8, TOK
        else:
            hd, dv, nkv, nqc = 128, 128, 2, SEQ
        qT = [sb("qT%d" % i, [hd, nqc], BF16) for i in range(2)]
        kT = [sb("kT%d" % i, [hd, TOK], BF16) for i in range(2)]
        v1 = [sb("v1%d" % i, [128, NT, nkv, dv + 1], BF16) for i in range(2)]
        pT = [sb("pT%d" % i, [128, NT, 512], BF16) for i in range(2)]
        den = sb("den", [128, 16], F32)
        osb = [sb("osb%d" % i, [128, 4, dv], BF16) for i in range(2)]
        psc = [pm("psc%d" % i, [128, 512], F32) for i in range(4)]
        po = [[pm("po%d_%d" % (i, h), [128, 2, dv + 1], F32) for h in range(2)] for i in range(2)]
        cnt = [0, 0]
        hi = 0
        vsrc = T["VB1"] if layer == 0 else T["VC1"]
        for b in range(NBC):
            p.dma("sp", v1[b][:], vsrc[b].rearrange("(t p) g d -> p t g d", p=128), [], ["v1%d" % b])
        pend = None
        for b in range(NBC):
            for h in range(8):
                q_ = qT[hi % 2]
                qn = "qT%d" % (hi % 2)
                if layer == 0:
                    k_ = kT[hi % 2]
                    kn = "kT%d" % (hi % 2)
                    p.dma("sp", q_[:], T["QBT"][b][:, h, :], [], [qn])
                    p.dma("sp", k_[:], T["KBT"][b][:, h, :], [], [kn])
                    hv = h
                    col0 = 512 + h * 64
                else:
                    g = h // 4
                    k_ = kT[(b * 2 + g) % 2]
                    kn = "kT%d" % ((b * 2 + g) % 2)
                    p.dma("sp", q_[:], T["QCT"][b][:, h, :], [], [qn])
                    if h % 4 == 0:
                        p.dma("sp", k_[:], T["KCT"][b][:, g, :], [], [kn])
                    hv = g
                    col0 = h * 128
                hi += 1
                chunks = [(c * 512, 512, list(range(NT))) for c in range(4)]
                if layer == 0:
                    chunks.append((SEQ, 256, [16, 17]))
                for (q0, nq, kts) in chunks:
                    o_ = osb[cnt[0] % 2]
                    on = "osb%d" % (cnt[0] % 2)
                    pvf = _attn_core_named(p, k_[:], kn, q_[:, q0:q0 + nq], qn, v1[b], "v1%d" % b, hv, kts, nq, dv, None,
                                           psc, pT, po, den, o_, on, None, cnt)
                    if pend is not None:
                        pend()

                    def pend(pvf=pvf, b=b, q0=q0, nq=nq, col0=col0, o_=o_, on=on):
                        pvf()
                        nj = nq // 128
                        p.dma("sp", T["OATT"][b][q0:q0 + nq, col0:col0 + dv].rearrange("(j p) d -> p j d", p=128),
                              o_[:, 0:nj, :], [on + "_0", on + "_1"], ["OATT"])
        pend()
        p.sc.emit()


def phase_o(nc, T, layer):
    with nc.cleanup_on_exit(), ExitStack() as es:
        sb = lambda name, shape, dt: es.enter_context(nc.sbuf_tensor("u%d_" % _U[0] + name, shape, dt))
        pm = lambda name, shape, dt: es.enter_context(nc.psum_tensor("u%d_" % _U[0] + name, shape, dt))
        p = Ph(nc)
        dsb = lambda name, shape, dt: Dbl(p, sb(name + "A", shape, dt), sb(name + "B", shape, dt))
        p.sc.dbl = {"oT", "t1_0", "t1_1", "xn_0", "xn_1", "junk", "ss", "ss_s", "ss_r", "h2", "h2T0", "h2T1", "lg", "v8", "sm0", "sm1", "sm2",
                    "ex", "sel", "pos", "ovf", "posb", "pos3", "ohk", "j32", "slotf0", "slotf1", "slotf2", "slotf3", "slotg"}
        identf, identb = _consts(p, es, nc, T)
        wout = sb("wout", [128, 8, 1024], BF16)
        g1 = sb("g1", [128, 3, 1024], F32)
        G2 = sb("G2", [128, 3, 1024], F32)
        S2 = sb("S2", [128, 3, 1024], F32)
        rw = sb("rw", [128, 8, 32], F32)
        rb = sb("rb", [128, 32], F32)
        ecap = sb("ecap", [128, 32], F32)
        trif = sb("trif", [128, 2, 128], F32)
        trib = sb("trib", [128, 2, 128], BF16)
        cntt = sb("cntt", [128, 32], F32)
        cnti = sb("cnti", [1, 32], I32)
        ot = [sb("ot%d" % i, [128, 1024], BF16) for i in range(2)]
        xt = [sb("xt%d" % i, [128, 1024], F32) for i in range(2)]
        oT = dsb("oT", [128, 8, 128], BF16)
        t1 = dsb("t1", [128, 1024], F32)
        xn_ = dsb("xn", [128, 1024], F32)
        junk = dsb("junk", [128, 1024], BF16)
        ss = dsb("ss", [128, 48], F32)
        h2 = dsb("h2", [128, 1024], F32)
        h2b = [sb("h2b%d" % i, [128, 1024], BF16) for i in range(2)]
        h2T = dsb("h2T", [128, 8, 128], F32)
        lg = dsb("lg", [128, 32], F32)
        v8 = dsb("v8", [128, 8], F32)
        sm = dsb("sm", [128, 8], F32)
        ex = dsb("ex", [128, 4], F32)
        gate = [sb("gate%d" % i, [128, 4], F32) for i in range(2)]
        sel = dsb("sel", [128, 32], BF16)
        pos = dsb("pos", [128, 32], F32)
        ovf = dsb("ovf", [128, 32], F32)
        posb = dsb("posb", [128, 32], F32)
        posc = dsb("posc", [128, 32], F32)
        ohk = dsb("ohk", [128, 32], F32)
        j32 = dsb("j32", [128, 32], F32)
        slotf = dsb("slotf", [128, 8], F32)
        sls = [sb("sls%d" % i, [128, 4], I32) for i in range(2)]
        slg = [sb("slg%d" % i, [128, 4], I32) for i in range(2)]
        ptr = pm("ptr", [128, 8, 128], BF16)
        pp = [pm("pp%d" % i, [128, 512], F32) for i in range(2)]
        ptf = pm("ptf", [128, 4, 128], F32)
        psl = pm("psl", [128, 32], F32)
        psr = pm("psr", [128, 2, 32], F32)
        wname = "ab_w_out" if layer == 0 else "c_w_out"
        p.sc.dma("pool", lambda e: e.dma_start(out=wout[:], in_=T[wname].rearrange("(k p) n -> p k n", p=128)), (), ["wout"])
        _load_mod(p, g1, T, layer, 2, "g1")
        _load_mod(p, S2, T, layer, 3, "S2")
        _load_mod(p, G2, T, layer, 4, "G2")
        p.dma("sp", rw[:], T["router_w"][layer].rearrange("(k p) e -> p k e", p=128), [], ["rw"])
        p.dma("sp", rb[:], T["router_b"][layer].partition_broadcast(128), [], ["rb"])
        p.dma("sp", ecap[:], T["ECAP"], [], ["ecap"])
        p.dma("sp", trif[:], T["TRI"].rearrange("m p q -> p m q"), [], ["trif"])
        p.cp("dve", trib[:], trif[:], ["trif"], ["trib"])
        p.memset("pool", cntt[:], 0.0, ["cnt"])
        ntl = NT if layer == 0 else 16
        tiles = [(b, t) for b in range(NBC) for t in range(ntl)]

        def xsrc(b, t):
            if layer == 0:
                if t < 16:
                    return T["x"][b, t * 128:(t + 1) * 128, :]
                return T["ctx"][b, (t - 16) * 128:(t - 15) * 128, :]
            return T["XR"][b, t * 128:(t + 1) * 128, :]

        def load(it):
            b, t = tiles[it]
            p.dma("sp", ot[it % 2][:], T["OATT"][b][t * 128:(t + 1) * 128, :], [], ["ot%d" % (it % 2)])
            p.dma("sp", xt[it % 2][:], xsrc(b, t), [], ["xt%d" % (it % 2)])

        load(0)
        for it, (b, t) in enumerate(tiles):
            p.sc.key = (it, 0) if PIPE else None
            if it + 1 < len(tiles):
                load(it + 1)
            p.setpar(it)
            j = b if t < 16 else 2
            rows = slice(t * 128, (t + 1) * 128)
            o_ = ot[it % 2]
            on = "ot%d" % (it % 2)
            x = xt[it % 2]
            xn = "xt%d" % (it % 2)
            hb_ = h2b[it % 2]
            hbn = "h2b%d" % (it % 2)
            ga = gate[it % 2]
            gan = "gate%d" % (it % 2)
            ss_ = sls[it % 2]
            ssn = "sls%d" % (it % 2)
            sg_ = slg[it % 2]
            sgn = "slg%d" % (it % 2)
            for k in range(8):
                p.tr(ptr[:, k, :], o_[:, k * 128:(k + 1) * 128], identb[:], [on, "identb"], ["ptr"])
            p.cp("act", oT[:], ptr[:], ["ptr"], ["oT"])
            for hf in range(2):
                for k in range(8):
                    p.mm(pp[hf][:], oT[:, k, :], wout[:, k, hf * 512:(hf + 1) * 512], k == 0, k == 7, ["oT", "wout"], ["pp%d" % hf])
            for hf in range(2):
                sl = slice(hf * 512, (hf + 1) * 512)
                p.tt("dve", t1[:, sl], pp[hf][:], g1[:, j, sl], ALU.mult, ["pp%d" % hf, "g1"], ["t1_%d" % hf])
                p.tt("pool", xn_[:, sl], t1[:, sl], x[:, sl], ALU.add, ["t1_%d" % hf, xn], ["xn_%d" % hf])
            p.dma("sp", T["XR"][b, rows, :], xn_[:], ["xn_0", "xn_1"], ["XR"])
            p.sc.key = (it, 1) if PIPE else None
            p.act(junk[:], xn_[:], AF.Square, ["xn_0", "xn_1"], ["junk", "ss"], accum=ss[:, 0:1])
            p.rstd(ss, 1, "ss", 1024)
            p.stt(t1[:], xn_[:], ss[:, 32:33], G2[:, j, :], ALU.mult, ALU.mult, ["xn_0", "xn_1", "ss_r", "G2", "t1_0", "t1_1"], ["t1_0", "t1_1"])
            p.tt("pool", h2[:], t1[:], S2[:, j, :], ALU.add, ["t1_0", "t1_1", "S2"], ["h2"])
            p.cp("act", hb_[:], h2[:], ["h2"], [hbn])
            for half in range(2):
                for k in range(4):
                    kk = half * 4 + k
                    p.tr(ptf[:, k, :], h2[:, kk * 128:(kk + 1) * 128], identf[:], ["h2", "identf"], ["ptf"])
                p.cp("dve", h2T[:, half * 4:half * 4 + 4, :], ptf[:], ["ptf"], ["h2T%d" % half])
            for k in range(8):
                p.mm(psl[:], h2T[:, k, :], rw[:, k, :], k == 0, k == 7, ["h2T0", "h2T1", "rw"], ["psl"])
            p.tt("dve", lg[:], psl[:], rb[:], ALU.add, ["psl", "rb"], ["lg"])
            p.sc.op("dve", lambda e, o_=v8[:], i_=lg[:]: e.max(out=o_, in_=i_), ["lg"], ["v8"])
            p.ts("dve", sm[:, 0:1], v8[:, 0:1], -1.0, None, ALU.mult, None, ["v8"], ["sm0"])
            p.act(ex[:], v8[:, 0:4], AF.Exp, ["v8", "sm0"], ["ex", "sm1"], bias=sm[:, 0:1], accum=sm[:, 1:2])
            p.rcp(sm[:, 2:3], sm[:, 1:2], ["sm1"], ["sm2"])
            p.ts("dve", ga[:], ex[:], sm[:, 2:3], None, ALU.mult, None, ["ex", "sm2"], [gan])
            p.ts("dve", sel[:], lg[:], v8[:, 3:4], None, ALU.is_ge, None, ["lg", "v8"], ["sel"])
            p.mm(psr[:, 0, :], trib[:, 0, :], sel[:], True, True, ["trib", "sel"], ["psr"])
            p.mm(psr[:, 1, :], trib[:, 1, :], sel[:], True, True, ["trib", "sel"], ["psr"])
            p.tt("dve", pos[:], psr[:, 0, :], cntt[:], ALU.add, ["psr", "cnt"], ["pos"])
            p.tt("dve", cntt[:], psr[:, 1, :], cntt[:], ALU.add, ["psr", "cnt"], ["cnt"])
            p.ts("dve", ovf[:], pos[:], float(CAP), BIG, ALU.is_ge, ALU.mult, ["pos"], ["ovf"])
            p.tt("dve", posb[:], pos[:], ecap[:], ALU.add, ["pos", "ecap"], ["posb"])
            p.tt("dve", posc[:], posb[:], ovf[:], ALU.add, ["posb", "ovf"], ["pos3"])
            for k in range(4):
                p.ts("dve", ohk[:], lg[:], v8[:, k:k + 1], None, ALU.is_equal, None, ["lg", "v8"], ["ohk"])
                p.stt(j32[:], ohk[:], 1.0, posc[:], ALU.mult, ALU.mult, ["ohk", "pos3"], ["j32", "slotf%d" % k], accum=slotf[:, k:k + 1])
            sfr = ["slotf%d" % k for k in range(4)]
            p.cp("dve", ss_[:], slotf[:, 0:4], sfr, [ssn])
            p.ts("dve", slotf[:, 4:8], slotf[:, 0:4], float(NSLOT), None, ALU.min, None, sfr, ["slotg"])
            p.cp("dve", sg_[:], slotf[:, 4:8], ["slotg"], [sgn])
            for k in range(4):
                p.sc.dma("pool", lambda e, k=k, ss_=ss_, hb_=hb_: e.indirect_dma_start(
                    out=T["XS"], out_offset=bass.IndirectOffsetOnAxis(ap=ss_[:, k:k + 1], axis=0),
                    in_=hb_[:], in_offset=None, bounds_check=p.reg(e, NSLOT - 1), oob_is_err=False), [hbn, ssn], ["XS%d" % k])
            p.dma("sp", T["SLOT"][b, rows, :], sg_[:], [sgn], ["SLOT"])
            p.dma("sp", T["GATE"][b, rows, :], ga[:], [gan], ["GATE"])
        p.sc.key = None
        p.dma("sp", T["CNT"][layer], cntt[:], ["cnt"], ["CNTd"])
        p.cp("dve", cnti[:], cntt[0:1, :], ["cnt"], ["cnti"])
        p.dma("sp", T["CNTI"][layer], cnti[:], ["cnti"], ["CNTId"])
        p.sc.emit()


def phase_e(nc, T, layer):
    with nc.cleanup_on_exit(), ExitStack() as es:
        sb = lambda name, shape, dt: es.enter_context(nc.sbuf_tensor("u%d_" % _U[0] + name, shape, dt))
        pm = lambda name, shape, dt: es.enter_context(nc.psum_tensor("u%d_" % _U[0] + name, shape, dt))
        p = Ph(nc)
        identf, identb = _consts(p, es, nc, T)
        win = [sb("win%d" % i, [128, 8, 2048], BF16) for i in range(2)]
        wout = [sb("wout%d" % i, [128, 8, 1024], BF16) for i in range(2)]
        bout = [sb("bout%d" % i, [128, 1024], F32) for i in range(2)]
        bsb = sb("bsb", [32, 2048], F32)
        bias = sb("bias", [128, 16, 32], F32)
        xg = [sb("xg%d" % i, [128, 1024], BF16) for i in range(2)]
        xT = [sb("xT%d" % i, [128, 8, 512], BF16) for i in range(2)]
        aT = [sb("aT%d" % i, [128, 8, 512], BF16) for i in range(2)]
        glu = sb("glu", [128, 512], F32)
        gluD = [glu, sb("glu1", [128, 512], F32)]
        sigD = [sb("sigD%d" % i, [128, 512], F32) for i in range(2)]
        linD = [sb("linD%d" % i, [128, 512], F32) for i in range(2)]
        sig = sb("sig", [128, 512], F32)
        lin = sb("lin", [128, 512], F32)
        gs = sb("gs", [128, 512], F32)
        lin2 = sb("lin2", [128, 512], F32)
        ysb = [sb("ysb%d" % i, [128, 1024], F32) for i in range(2)]
        ptr = pm("ptr", [128, 8, 128], BF16)
        pg = [pm("pg%d" % i, [128, 512], F32) for i in range(2)]
        pl = [pm("pl%d" % i, [128, 512], F32) for i in range(2)]
        py = [pm("py%d" % i, [128, 512], F32) for i in range(2)]
        ptb = pm("ptb", [128, 16, 32], F32)
        p.dma("sp", bsb[:], T["moe_b_in"][layer], [], ["bsb"])
        bv = bsb[:].rearrange("e (fc p two) -> e fc two p", p=128, two=2)
        for two in range(2):
            for fc in range(8):
                p.tr(ptb[:, two * 8 + fc, :], bv[:, fc, two, :], identf[0:32, 0:32], ["bsb", "identf"], ["ptb"])
        p.cp("dve", bias[:], ptb[:], ["ptb"], ["bias"])
        p.ts("dve", bias[:, 8:16, :], bias[:, 8:16, :], 1.0, None, ALU.add, None, ["bias"], ["bias"])

        def loadw(e):
            w_ = win[e % 2]
            o_ = wout[e % 2]
            src = T["moe_w_in"][layer, e].rearrange("(k p) n -> p k n", p=128)
            for q in range(4):
                p.sc.dma("pool", lambda en, w_=w_, src=src, q=q: en.dma_start(out=w_[:, 2 * q:2 * q + 2, :], in_=src[:, 2 * q:2 * q + 2, :]), (), ["win%d_%d" % (e % 2, q)])
            src2 = T["moe_w_out"][layer, e].rearrange("(k p) n -> p k n", p=128)
            for q in range(2):
                p.sc.dma("pool", lambda en, o_=o_, src2=src2, q=q: en.dma_start(out=o_[:, 4 * q:4 * q + 4, :], in_=src2[:, 4 * q:4 * q + 4, :]), (), ["wout%d_%d" % (e % 2, q)])
            p.dma("sp", bout[e % 2][:], T["moe_b_out"][layer, e].partition_broadcast(128), [], ["bout%d" % (e % 2)])

        cs = sb("cs", [1, NE], I32)
        p.sc.cnt_ap = lambda key: cs[0:1, key:key + 1]
        p.sc.cnt_dep = p.sc.dma("sp", lambda e_: e_.dma_start(out=cs[:], in_=T["CNTI"][layer]), (), ["cs"])
        for i in range(2):
            p.memset("dve", xT[i][:], 0.0, ["xT%d_%d" % (i, tt) for tt in range(4)])
        loadw(0)
        if os.environ.get("KPROBE") == "noweights":
            loadw(1)
            loadw = lambda e: None
        groups = [(e, grp) for e in range(NE) for grp in range(CAP // 512)]
        st = {"ti": 0, "fi": 0, "yi": 0}

        def stage_l(g):
            e, grp = groups[g]
            base = grp * 512
            xT_ = xT[g % 2]
            xTn = "xT%d" % (g % 2)
            for tt in range(4):
                p.sc.cur_cond = (e, base + tt * 128)
                s0 = e * CAP + base + tt * 128
                x_ = xg[st["ti"] % 2]
                xn = "xg%d" % (st["ti"] % 2)
                st["ti"] += 1
                p.dma("sp", x_[:], T["XS"][s0:s0 + 128, :], [], [xn])
                for k in range(8):
                    p.tr(ptr[:, k, :], x_[:, k * 128:(k + 1) * 128], identb[:], [xn, "identb"], ["ptr"])
                p.cp("dve", xT_[:, :, tt * 128:(tt + 1) * 128], ptr[:], ["ptr"], [xTn + "_%d" % tt])
                p.sc.cur_cond = None

        def stage_c(g):
            e, grp = groups[g]
            base = grp * 512
            xT_ = xT[g % 2]
            xTn = "xT%d" % (g % 2)
            aT_ = aT[g % 2]
            aTn = "aT%d" % (g % 2)
            w_ = win[e % 2]
            wv = w_[:].rearrange("p k (f two) -> p k f two", two=2)
            wr = ["win%d_%d" % (e % 2, q) for q in range(4)]
            fin = None
            for hh in range(2):
                p.sc.cur_cond = (e, base + hh * 256)
                c_ = slice(hh * 256, (hh + 1) * 256)
                xr = [xTn + "_%d" % tt for tt in (2 * hh, 2 * hh + 1)]
                for fc in range(8):
                    fi = st["fi"]
                    st["fi"] += 1
                    pg_ = pg[fi % 2]
                    pl_ = pl[fi % 2]
                    pgn = "pg%d" % (fi % 2)
                    pln = "pl%d" % (fi % 2)
                    for k in range(8):
                        p.mm(pg_[:, 0:256], wv[:, k, fc * 128:(fc + 1) * 128, 0], xT_[:, k, c_], k == 0, k == 7, wr + xr, [pgn])
                    for k in range(8):
                        p.mm(pl_[:, 0:256], wv[:, k, fc * 128:(fc + 1) * 128, 1], xT_[:, k, c_], k == 0, k == 7, wr + xr, [pln])
                    b2 = fi % 2
                    glu_ = gluD[b2]
                    sig_ = sigD[b2]
                    lin_ = linD[b2]
                    p.ts("dve", glu_[:, 0:256], pg_[:, 0:256], bias[:, fc, e:e + 1], 7.0, ALU.add, ALU.min, [pgn, "bias"], ["glu%d" % b2])
                    p.act(sig_[:, 0:256], glu_[:, 0:256], AF.Sigmoid, ["glu%d" % b2], ["sig%d" % b2], scale=1.702)
                    p.ts("dve", lin_[:, 0:256], pl_[:, 0:256], bias[:, 8 + fc, e:e + 1], 8.0, ALU.add, ALU.min, [pln, "bias"], ["lin%d" % b2])
                    if fin is not None:
                        fin()

                    def fin(glu_=glu_, sig_=sig_, lin_=lin_, b2=b2, fc=fc, hh=hh, c_=c_):
                        p.tt("dve", gs[:, 0:256], glu_[:, 0:256], sig_[:, 0:256], ALU.mult, ["glu%d" % b2, "sig%d" % b2], ["gs"])
                        p.stt(aT_[:, fc, c_], lin_[:, 0:256], -6.0, gs[:, 0:256], ALU.max, ALU.mult, ["gs", "lin%d" % b2], [aTn + "_%d_%d" % (fc, hh)])
                fin()
                fin = None
                p.sc.cur_cond = None

        def stage_o(g):
            e, grp = groups[g]
            base = grp * 512
            aT_ = aT[g % 2]
            aTn = "aT%d" % (g % 2)
            o_ = wout[e % 2]
            wor = ["wout%d_%d" % (e % 2, q) for q in range(2)]
            for tt in range(4):
                p.sc.cur_cond = (e, base + tt * 128)
                s0 = e * CAP + base + tt * 128
                ar = [aTn + "_%d_%d" % (fc, tt // 2) for fc in range(8)]
                y_ = ysb[st["yi"] % 2]
                yn = "ysb%d" % (st["yi"] % 2)
                st["yi"] += 1
                for hf in range(2):
                    for fc in range(8):
                        p.mm(py[hf][:], aT_[:, fc, tt * 128:(tt + 1) * 128], o_[:, fc, hf * 512:(hf + 1) * 512], fc == 0, fc == 7, ar + wor, ["py%d" % hf])
                    p.tt("dve", y_[:, hf * 512:(hf + 1) * 512], py[hf][:], bout[e % 2][:, hf * 512:(hf + 1) * 512], ALU.add,
                         ["py%d" % hf, "bout%d" % (e % 2)], [yn + "_%d" % hf])
                p.dma("sp", T["YS"][s0:s0 + 128, :], y_[:], [yn + "_0", yn + "_1"], ["YS"])
                p.sc.cur_cond = None

        stage_l(0)
        for g, (e, grp) in enumerate(groups):
            if grp == 0 and e + 1 < NE:
                loadw(e + 1)
            stage_c(g)
            if g + 1 < len(groups):
                stage_l(g + 1)
            stage_o(g)
        p.sc.emit()


def phase_c(nc, T, layer):
    with nc.cleanup_on_exit(), ExitStack() as es:
        sb = lambda name, shape, dt: es.enter_context(nc.sbuf_tensor("u%d_" % _U[0] + name, shape, dt))
        p = Ph(nc)
        epst = sb("epst", [128, 1], F32)
        p.memset("pool", epst[:], EPS, ["eps"])
        p.eps_ap = epst[:, 0:1]
        g2 = sb("g2", [128, 3, 1024], F32)
        gfin = sb("gfin", [128, 1024], F32)
        _load_mod(p, g2, T, layer, 5, "g2")
        p.dma("sp", gfin[:], T["final_norm_g"].partition_broadcast(128), [], ["gfin"])
        yk = [[sb("yk%d_%d" % (i, k), [128, 1024], F32) for k in range(4)] for i in range(2)]
        xt = [sb("xt%d" % i, [128, 1024], F32) for i in range(2)]
        sl = [sb("sl%d" % i, [128, 4], I32) for i in range(2)]
        ga = [sb("ga%d" % i, [128, 4], F32) for i in range(2)]
        acc = sb("acc", [128, 1024], F32)
        t1 = sb("t1", [128, 1024], F32)
        xo = [sb("xo%d" % i, [128, 1024], F32) for i in range(2)]
        junk = sb("junk", [128, 1024], BF16)
        ss = sb("ss", [128, 48], F32)
        oo = [sb("oo%d" % i, [128, 1024], F32) for i in range(2)]
        ntl = NT if layer == 0 else 16
        tiles = [(b, t) for b in range(NBC) for t in range(ntl)]

        def load(it):
            b, t = tiles[it]
            rows = slice(t * 128, (t + 1) * 128)
            i2 = it % 2
            p.dma("sp", sl[i2][:], T["SLOT"][b, rows, :], [], ["sl%d" % i2])
            p.dma("sp", ga[i2][:], T["GATE"][b, rows, :], [], ["ga%d" % i2])
            p.dma("sp", xt[i2][:], T["XR"][b, rows, :], [], ["xt%d" % i2])
            for k in range(4):
                p.sc.dma("pool", lambda e, k=k, i2=i2: e.indirect_dma_start(
                    out=yk[i2][k][:], out_offset=None, in_=T["YS"],
                    in_offset=bass.IndirectOffsetOnAxis(ap=sl[i2][:, k:k + 1], axis=0)), ["sl%d" % i2], ["yk%d_%d" % (i2, k)])

        load(0)
        for it, (b, t) in enumerate(tiles):
            if it + 1 < len(tiles):
                load(it + 1)
            i2 = it % 2
            j = b if t < 16 else 2
            rows = slice(t * 128, (t + 1) * 128)
            ykr = ["yk%d_%d" % (i2, k) for k in range(4)]
            p.ts("dve", acc[:], yk[i2][0][:], ga[i2][:, 0:1], None, ALU.mult, None, [ykr[0], "ga%d" % i2], ["acc"])
            for k in range(1, 4):
                p.stt(acc[:], yk[i2][k][:], ga[i2][:, k:k + 1], acc[:], ALU.mult, ALU.add, [ykr[k], "ga%d" % i2, "acc"], ["acc"])
            p.tt("pool", t1[:], acc[:], g2[:, j, :], ALU.mult, ["acc", "g2"], ["t1"])
            x_ = xo[i2]
            xon = "xo%d" % i2
            p.tt("dve", x_[:], t1[:], xt[i2][:], ALU.add, ["t1", "xt%d" % i2], [xon])
            if layer == 0:
                p.dma("sp", T["XR"][b, rows, :], x_[:], [xon], ["XRw"])
            else:
                p.act(junk[:], x_[:], AF.Square, [xon], ["junk", "ss"], accum=ss[:, 0:1])
                p.rstd(ss, 1, "ss", 1024)
                p.stt(oo[i2][:], x_[:], ss[:, 32:33], gfin[:], ALU.mult, ALU.mult, [xon, "ss_r", "gfin"], ["oo%d" % i2])
                p.dma("sp", T["out"][b, rows, :], oo[i2][:], ["oo%d" % i2], ["outw"])
        p.sc.emit()


_IN_SHAPES = {
    "x": ([NBC, SEQ, D], F32), "ctx": ([NBC, NCTX, D], F32), "c": ([NBC, D], F32), "c_ctx": ([D], F32),
    "ada_w": ([2, D, 6 * D], F32), "ada_b": ([2, 6 * D], F32), "norm_mix_g": ([2, D], F32), "norm_ffn_g": ([2, D], F32),
    "ab_w_in": ([D, 1440], F32), "mla_q_norm_g": ([384], F32), "mla_wq_b": ([384, 768], F32),
    "mla_kv_norm_g": ([256], F32), "mla_wkv_b": ([256, 1024], F32), "swa_sink": ([8], F32),
    "ab_w_out": ([D, D], F32), "c_w_in": ([D, 1536], F32), "c_q_norm_g": ([128], F32), "c_k_norm_g": ([128], F32),
    "c_w_out": ([D, D], F32), "router_w": ([2, D, NE], F32), "router_b": ([2, NE], F32),
    "moe_w_in": ([2, NE, D, 2 * D], F32), "moe_b_in": ([2, NE, 2 * D], F32),
    "moe_w_out": ([2, NE, D, D], F32), "moe_b_out": ([2, NE, D], F32), "final_norm_g": ([D], F32),
    "IDENT": ([128, 128], F32), "MASKS": ([2, 128, 128], F32), "TRI": ([2, 128, 128], F32), "ECAP": ([128, NE], F32),
    "ROPE0": ([SEQ, 96], F32), "ROPE1": ([SEQ, 128], F32),
}
_SCRATCH = {
    "MOD": ([2, 3, 6 * D], F32), "XR": ([NBC, TOK, D], F32),
    "QAT": ([NBC, 128, 4, TOK], BF16), "KAT": ([NBC, 128, TOK], BF16), "VA1": ([NBC, TOK, 2, 65], BF16),
    "QBT": ([NBC, 96, 8, TOK], BF16), "KBT": ([NBC, 96, 8, TOK], BF16), "VB1": ([NBC, TOK, 8, 65], BF16),
    "QCT": ([NBC, 128, 8, SEQ], BF16), "KCT": ([NBC, 128, 2, TOK], BF16), "VC1": ([NBC, TOK, 2, 129], BF16),
    "OATT": ([NBC, TOK, D], BF16), "XS": ([NSLOT, D], BF16), "YS": ([NSLOT + 128, D], F32),
    "SLOT": ([NBC, TOK, 4], I32), "GATE": ([NBC, TOK, 4], F32), "CNT": ([2, 128, NE], F32), "CNTI": ([2, 1, NE], I32),
}


def build_program(stop_after=None, debug=()):
    nc = bass.Bass("TRN2", target_bir_lowering=False)
    T = {}
    for k, (shp, dt) in _IN_SHAPES.items():
        T[k] = nc.dram_tensor(k, shp, dt, kind="ExternalInput").ap()
    for k, (shp, dt) in _SCRATCH.items():
        kind = "ExternalOutput" if k in debug else "Internal"
        T[k] = nc.dram_tensor(k, shp, dt, kind=kind).ap()
    T["out"] = nc.dram_tensor("out", [NBC, SEQ, D], F32, kind="ExternalOutput").ap()
    phases = [
        ("mod", lambda: phase_mod(nc, T)),
        ("p1_0", lambda: phase_p1(nc, T, 0)),
        ("aa", lambda: phase_attn_a(nc, T)),
        ("ab", lambda: phase_attn_dense(nc, T, 0)),
        ("o_0", lambda: phase_o(nc, T, 0)),
        ("e_0", lambda: phase_e(nc, T, 0)),
        ("c_0", lambda: phase_c(nc, T, 0)),
        ("p1_1", lambda: phase_p1(nc, T, 1)),
        ("ac", lambda: phase_attn_dense(nc, T, 1)),
        ("o_1", lambda: phase_o(nc, T, 1)),
        ("e_1", lambda: phase_e(nc, T, 1)),
        ("c_1", lambda: phase_c(nc, T, 1)),
    ]
    for name, fn in phases:
        fn()
        if stop_after == name:
            break
    return nc


def _rope_tab(rot_dim):
    t = np.arange(SEQ)
    rows = (t // 64).astype(np.float32)
    cols = (t % 64).astype(np.float32)
    quarter = rot_dim // 4
    inv = (np.float32(10000.0) ** (-np.arange(quarter, dtype=np.float32) / np.float32(quarter))).astype(np.float32)
    ang = np.concatenate([rows[:, None] * inv, cols[:, None] * inv], axis=-1).astype(np.float32)
    return np.cos(ang).astype(np.float32), np.sin(ang).astype(np.float32)


def _const_inputs():
    ca, sa = _rope_tab(64)
    cb, sb_ = _rope_tab(32)
    cc, sc_ = _rope_tab(128)
    pi = np.arange(128)
    masks = np.stack([(pi[:, None] >= pi[None, :]), (pi[:, None] <= pi[None, :])]).astype(np.float32)
    tri = np.stack([(pi[:, None] < pi[None, :]), np.ones((128, 128), bool)]).astype(np.float32)
    ecap = np.broadcast_to((np.arange(NE) * CAP).astype(np.float32)[None, :], (128, NE)).copy()
    return {
        "IDENT": np.eye(128, dtype=np.float32), "MASKS": masks, "TRI": tri, "ECAP": ecap,
        "ROPE0": np.ascontiguousarray(np.concatenate([ca, sa, cb, sb_], axis=1)),
        "ROPE1": np.ascontiguousarray(np.concatenate([cc, sc_], axis=1)),
    }


def make_in_maps(inputs, n_cores=8):
    consts = _const_inputs()
    sq = {"ab_w_in", "mla_q_norm_g", "mla_wq_b", "mla_kv_norm_g", "mla_wkv_b", "swa_sink", "ab_w_out",
          "c_w_in", "c_q_norm_g", "c_k_norm_g", "c_w_out"}
    shared = {}
    for k in _IN_SHAPES:
        if k in consts:
            shared[k] = consts[k]
        elif k in ("x", "ctx", "c"):
            continue
        else:
            a = np.asarray(inputs[k], dtype=np.float32)
            if k in sq:
                a = a[0]
            shared[k] = np.ascontiguousarray(a)
    maps = []
    for c in range(n_cores):
        m = dict(shared)
        for k in ("x", "ctx", "c"):
            m[k] = np.ascontiguousarray(np.asarray(inputs[k], dtype=np.float32)[c * NBC:(c + 1) * NBC])
        maps.append(m)
    return maps


def kernel(**inputs):
    nc = build_program()
    maps = make_in_maps(inputs, 8)
    res = run_bass_kernel_spmd(nc, maps, core_ids=list(range(8)))
    return np.concatenate([np.asarray(r["out"]) for r in res.results], axis=0).astype(np.float32)
```

```python
import concourse.bass as bass
import concourse.mybir as mybir

F32 = mybir.dt.float32
BF16 = mybir.dt.bfloat16
I32 = mybir.dt.int32
U32 = mybir.dt.uint32
ALU = mybir.AluOpType
AF = mybir.ActivationFunctionType
AX = mybir.AxisListType


class _Op:
    __slots__ = ("eng", "fn", "deps", "dma", "needed", "sig", "idx", "thr", "cond", "key")

    def __init__(self, eng, fn, dma):
        self.cond = None
        self.key = None
        self.eng = eng
        self.fn = fn
        self.dma = dma
        self.deps = []
        self.needed = dma
        self.sig = None
        self.thr = None


class Sched:
    ENGS = ("pe", "act", "dve", "pool", "sp")
    KDMA = 8

    def __init__(self, nc):
        self.nc = nc
        self.ops = []
        self.lastw = {}
        self.readers = {}
        self.cur_cond = None
        self.cnt_ap = None
        self.cnt_dep = None
        self.key = None
        self.dbl = set()
        self.sfx = ""

    def _add(self, eng, fn, reads, writes, dma):
        if self.dbl:
            reads = tuple(r + self.sfx if r in self.dbl else r for r in reads)
            writes = tuple(r + self.sfx if r in self.dbl else r for r in writes)
        op = _Op(eng, fn, dma)
        deps = {}
        for r in reads:
            w = self.lastw.get(r)
            if w is not None:
                deps[id(w)] = w
        for r in writes:
            w = self.lastw.get(r)
            if w is not None:
                deps[id(w)] = w
            for rd in self.readers.get(r, ()):
                deps[id(rd)] = rd
        for r in writes:
            self.lastw[r] = op
            self.readers[r] = []
        for r in reads:
            if r not in writes:
                self.readers.setdefault(r, []).append(op)
        for d in deps.values():
            if d is op:
                continue
            if d.eng == "pe" and eng == "pe" and not d.dma:
                continue
            d.needed = True
            op.deps.append(d)
        op.cond = self.cur_cond
        op.key = self.key
        self.ops.append(op)
        return op

    def op(self, eng, fn, reads=(), writes=()):
        return self._add(eng, fn, tuple(reads), tuple(writes), False)

    def dma(self, queue, fn, reads=(), writes=()):
        return self._add(queue, fn, tuple(reads), tuple(writes), True)

    def emit(self):
        nc = self.nc
        from contextlib import ExitStack

        with ExitStack() as es:
            Sched._uid = getattr(Sched, "_uid", 0) + 1
            u = "p%d_" % Sched._uid
            esem = {e: nc.alloc_semaphore(name=u + "s_" + e) for e in self.ENGS}
            qsem = {
                q: [nc.alloc_semaphore(name=u + "d_%s%d" % (q, i)) for i in range(self.KDMA)]
                for q in ("sp", "pool")
            }
            ecount = {e: 0 for e in self.ENGS}
            qn = {q: 0 for q in qsem}
            per = {e: [o for o in self.ops if o.eng == e] for e in self.ENGS}
            if any(o.key is not None for o in self.ops):
                seen = False
                last = max(i for i, o in enumerate(self.ops) if o.key is not None)
                sk = {}
                for i, o in enumerate(self.ops):
                    if o.key is not None:
                        seen = True
                        sk[id(o)] = float(o.key[0] + o.key[1])
                    else:
                        sk[id(o)] = float("inf") if (seen and i > last) else (float("-inf") if not seen else None)
                        assert sk[id(o)] is not None, "unkeyed op inside a keyed loop"
                groups = {}
                for o in self.ops:
                    groups.setdefault(sk[id(o)], []).append(o)
                glob = []
                for k in sorted(groups):
                    g = groups[k]
                    tiles = []
                    for o in g:
                        t = o.key[0] if o.key is not None else None
                        if t not in tiles:
                            tiles.append(t)
                    if len(tiles) <= 1:
                        glob.extend(g)
                        continue
                    items = []
                    for ti, t in enumerate(tiles):
                        seq = [o for o in g if (o.key[0] if o.key is not None else None) == t]
                        for i, o in enumerate(seq):
                            items.append(((i + 0.5) / len(seq), ti, i, o))
                    items.sort(key=lambda x: (x[0], x[1], x[2]))
                    glob.extend(x[3] for x in items)
                per = {e: [o for o in glob if o.eng == e] for e in self.ENGS}
            for e in self.ENGS:
                for op in per[e]:
                    if op.dma:
                        i = qn[op.eng]
                        qn[op.eng] += 1
                        s = qsem[op.eng][i % self.KDMA]
                        op.sig = (s, 16 * (i // self.KDMA + 1))
                        if i >= self.KDMA:
                            op.thr = (s, 16 * (i // self.KDMA))
                    elif op.needed:
                        ecount[op.eng] += 1
                        op.sig = (esem[op.eng], ecount[op.eng])
            block = es.enter_context(nc.Block())

            def run(ename, eng):
                waited = {}

                def emit_op(op, waited):
                    ws = [d.sig for d in op.deps]
                    if op.thr is not None:
                        ws.append(op.thr)
                    for (s, v) in ws:
                        k = id(s)
                        if waited.get(k, 0) >= v:
                            continue
                        waited[k] = v
                        eng.wait_ge(s, v)
                    ins = op.fn(eng)
                    if op.sig is not None:
                        ins.then_inc(op.sig[0], 16 if op.dma else 1)

                ops = per[ename]
                i = 0
                creg = None
                curkey = None
                while i < len(ops):
                    c = ops[i].cond
                    j = i
                    while j < len(ops) and ops[j].cond == c:
                        j += 1
                    seg = ops[i:j]
                    i = j
                    if c is None:
                        for op in seg:
                            emit_op(op, waited)
                        continue
                    key, thr = c[0], c[1]
                    hi = c[2] if len(c) > 2 else None
                    if creg is None:
                        creg = eng.alloc_register("creg_" + u + ename)
                        d = self.cnt_dep
                        if d is not None and waited.get(id(d.sig[0]), 0) < d.sig[1]:
                            waited[id(d.sig[0])] = d.sig[1]
                            eng.wait_ge(d.sig[0], d.sig[1])
                    if curkey != key:
                        eng.reg_load(creg, self.cnt_ap(key))
                        curkey = key
                    saved = dict(waited)

                    def comp():
                        w2 = dict(saved)
                        ncomp = sum(1 for op in seg if (not op.dma) and op.sig is not None)
                        if ncomp:
                            eng.drain()
                            eng.sem_inc(esem[ename], ncomp)
                        for op in seg:
                            if op.dma:
                                if op.thr is not None and w2.get(id(op.thr[0]), 0) < op.thr[1]:
                                    w2[id(op.thr[0])] = op.thr[1]
                                    eng.wait_ge(op.thr[0], op.thr[1])
                                eng.sem_inc(op.sig[0], 16)

                    def body():
                        w3 = dict(saved)
                        for op in seg:
                            emit_op(op, w3)

                    with eng.If_lt(creg, thr + 1):
                        comp()
                    with eng.Else():
                        if hi is None:
                            body()
                        else:
                            with eng.If_lt(creg, hi + 1):
                                body()
                            with eng.Else():
                                comp()
                    waited = saved
                if ename in qsem:
                    n = qn[ename]
                    for i2, s in enumerate(qsem[ename]):
                        cnt = (n - i2 + self.KDMA - 1) // self.KDMA if n > i2 else 0
                        if cnt > 0 and waited.get(id(s), 0) < 16 * cnt:
                            eng.wait_ge(s, 16 * cnt)

            @block.sync
            def _(e):
                run("sp", e)

            @block.gpsimd
            def _(e):
                run("pool", e)

            @block.scalar
            def _(e):
                run("act", e)

            @block.vector
            def _(e):
                run("dve", e)

            @block.tensor
            def _(e):
                run("pe", e)

import os
import numpy as np
from contextlib import ExitStack
from concourse.bass_utils import run_bass_kernel_spmd

NBC = 2
SEQ = 2048
NCTX = 256
TOK = SEQ + NCTX
NT = TOK // 128
D = 1024
NE = 32
CAP = 1536
NSLOT = NE * CAP
EPS = 1e-6
BIG = 1.0e6
NOPOOL = True
PIPE = True
NOACTCP = True


_U = [0]


class Dbl:
    def __init__(self, p, a, b):
        self.p = p
        self.t = (a, b)

    def __getitem__(self, k):
        return self.t[self.p.par][k]


class Ph:
    def __init__(self, nc):
        _U[0] += 1
        self.nc = nc
        self.par = 0
        self.nopool = NOPOOL
        self.sc = Sched(nc)

    def setpar(self, it):
        self.par = it % 2
        self.sc.sfx = "#%d" % (it % 2)

    def reg(self, e, val):
        if not hasattr(self, "_regs"):
            self._regs = {}
        if val not in self._regs:
            self._regs[val] = e.to_reg(val)
        return self._regs[val]

    def mm(self, out, lhsT, rhs, start, stop, r, w):
        self.sc.op("pe", lambda e: e.matmul(out, lhsT=lhsT, rhs=rhs, start=start, stop=stop), r, w)

    def tr(self, out, in_, ident, r, w):
        self.sc.op("pe", lambda e: e.transpose(out=out, in_=in_, identity=ident), r, w)

    def act(self, out, in_, func, r, w, scale=1.0, bias=None, accum=None):
        kw = {}
        if NOACTCP and func == AF.Copy and accum is None and bias is None:
            self.sc.op("dve", lambda e: e.tensor_scalar(out=out, in0=in_, scalar1=float(scale), scalar2=None, op0=ALU.mult), r, w)
            return
        if accum is not None:
            kw["accum_out"] = accum
        if bias is not None:
            kw["bias"] = bias
        self.sc.op("act", lambda e: e.activation(out=out, in_=in_, func=func, scale=scale, **kw), r, w)

    def tt(self, eng, out, a, b, op, r, w):
        eng = "dve" if (eng == "pool" and self.nopool) else eng
        self.sc.op(eng, lambda e: e.tensor_tensor(out=out, in0=a, in1=b, op=op), r, w)

    def ts(self, eng, out, a, s1, s2, op0, op1, r, w):
        eng = "dve" if (eng == "pool" and self.nopool) else eng
        if s2 is None:
            self.sc.op(eng, lambda e: e.tensor_scalar(out=out, in0=a, scalar1=s1, scalar2=None, op0=op0), r, w)
        else:
            self.sc.op(eng, lambda e: e.tensor_scalar(out=out, in0=a, scalar1=s1, scalar2=s2, op0=op0, op1=op1), r, w)

    def stt(self, out, in0, scalar, in1, op0, op1, r, w, accum=None):
        kw = {}
        if accum is not None:
            kw["accum_out"] = accum
        self.sc.op("dve", lambda e: e.scalar_tensor_tensor(out=out, in0=in0, scalar=scalar, in1=in1, op0=op0, op1=op1, **kw), r, w)

    def cp(self, eng, out, in_, r, w):
        eng = "dve" if (eng == "pool" and self.nopool) else eng
        if eng == "act" and NOACTCP:
            eng = "dve"
        if eng == "act":
            self.sc.op(eng, lambda e: e.copy(out=out, in_=in_), r, w)
        else:
            self.sc.op(eng, lambda e: e.tensor_copy(out=out, in_=in_), r, w)

    def rcp(self, out, in_, r, w):
        self.sc.op("dve", lambda e: e.reciprocal(out=out, in_=in_), r, w)

    def dma(self, q, out, in_, r, w):
        self.sc.dma(q, lambda e: e.dma_start(out=out, in_=in_), r, w)

    def memset(self, eng, ap, val, w):
        eng = "dve" if (eng == "pool" and self.nopool) else eng
        self.sc.op(eng, lambda e: e.memset(ap, val), (), w)

    def rstd(self, ss, n, name, dim):
        self.act(ss[:, 16:17], ss[:, 0:1], AF.Sqrt, [name], [name + "_s"], scale=1.0 / dim, bias=self.eps_ap)
        self.rcp(ss[:, 32:33], ss[:, 16:17], [name + "_s"], [name + "_r"])


def _consts(p, es, nc, T):
    sb = lambda name, shape, dt: es.enter_context(nc.sbuf_tensor("u%d_" % _U[0] + name, shape, dt))
    identf = sb("identf", [128, 128], F32)
    identb = sb("identb", [128, 128], BF16)
    epst = sb("epst", [128, 1], F32)
    p.dma("sp", identf[:], T["IDENT"], [], ["identf"])
    p.cp("dve", identb[:], identf[:], ["identf"], ["identb"])
    p.memset("pool", epst[:], EPS, ["eps"])
    p.eps_ap = epst[:, 0:1]
    return identf, identb


def phase_mod(nc, T):
    with nc.cleanup_on_exit(), ExitStack() as es:
        sb = lambda name, shape, dt: es.enter_context(nc.sbuf_tensor("u%d_" % _U[0] + name, shape, dt))
        pm = lambda name, shape, dt: es.enter_context(nc.psum_tensor("u%d_" % _U[0] + name, shape, dt))
        p = Ph(nc)
        cT = sb("cT", [128, 3, 8], F32)
        wb = [sb("wb%d" % i, [128, 8, 512], F32) for i in range(2)]
        adab = sb("adab", [3, 6144], F32)
        ng = sb("ng", [3, 2, 1024], F32)
        mod = sb("mod", [3, 6144], F32)
        tmp = sb("tmp", [3, 512], F32)
        ps = [pm("psm%d" % i, [128, 512], F32) for i in range(2)]
        for j in range(2):
            p.dma("sp", cT[:, j, :], T["c"][j].rearrange("(p k) -> p k", k=8), [], ["cT"])
        p.dma("sp", cT[:, 2, :], T["c_ctx"].rearrange("(p k) -> p k", k=8), [], ["cT"])
        p.act(cT[:], cT[:], AF.Silu, ["cT"], ["cT"])
        it = 0
        for i in range(2):
            p.dma("sp", adab[:], T["ada_b"][i].partition_broadcast(3), [], ["adab"])
            p.dma("sp", ng[:, 0, :], T["norm_mix_g"][i].partition_broadcast(3), [], ["ng"])
            p.dma("sp", ng[:, 1, :], T["norm_ffn_g"][i].partition_broadcast(3), [], ["ng"])
            wv = T["ada_w"][i].rearrange("(p k) n -> p k n", k=8)
            for n in range(12):
                w_ = wb[it % 2]
                wn = "wb%d" % (it % 2)
                pn = "ps%d" % (it % 2)
                pp = ps[it % 2]
                it += 1
                p.dma("sp", w_[:], wv[:, :, n * 512:(n + 1) * 512], [], [wn])
                for k in range(8):
                    p.mm(pp[0:3, :], cT[:, :, k], w_[:, k, :], k == 0, k == 7, ["cT", wn], [pn])
                sl = slice(n * 512, (n + 1) * 512)
                if n in (2, 3, 8, 9):
                    gi = 0 if n < 4 else 1
                    go = (n % 2) * 512
                    p.tt("dve", tmp[:], pp[0:3, :], adab[:, sl], ALU.add, [pn, "adab"], ["tmp"])
                    p.stt(mod[:, sl], tmp[:], 1.0, ng[:, gi, go:go + 512], ALU.add, ALU.mult, ["tmp", "ng"], ["mod"])
                else:
                    p.tt("dve", mod[:, sl], pp[0:3, :], adab[:, sl], ALU.add, [pn, "adab"], ["mod"])
            p.dma("sp", T["MOD"][i], mod[:], ["mod"], ["MODd"])
        p.sc.emit()


def _load_mod(p, tile, T, layer, idx, name):
    for j in range(3):
        p.dma("sp", tile[:, j, :], T["MOD"][layer, j, idx * 1024:(idx + 1) * 1024].partition_broadcast(128), [], [name])


def _rope(p, x1, x2, cos, sin, o1, o2, tmps, rd, wr1, wr2):
    ra, rb, rc, rd_ = tmps
    p.tt("dve", ra, x1, cos, ALU.mult, rd, ["ra"])
    p.tt("pool", rb, x2, sin, ALU.mult, rd, ["rb"])
    p.tt("dve", o1, ra, rb, ALU.subtract, ["ra", "rb"], [wr1])
    p.tt("pool", rc, x2, cos, ALU.mult, rd, ["rc"])
    p.tt("dve", rd_, x1, sin, ALU.mult, rd, ["rd"])
    p.tt("pool", o2, rc, rd_, ALU.add, ["rc", "rd"], [wr2])


def phase_p1(nc, T, layer):
    with nc.cleanup_on_exit(), ExitStack() as es:
        sb = lambda name, shape, dt: es.enter_context(nc.sbuf_tensor("u%d_" % _U[0] + name, shape, dt))
        pm = lambda name, shape, dt: es.enter_context(nc.psum_tensor("u%d_" % _U[0] + name, shape, dt))
        p = Ph(nc)
        dsb = lambda name, shape, dt: Dbl(p, sb(name + "A", shape, dt), sb(name + "B", shape, dt))
        p.sc.dbl = {"junk", "ss", "ss_s", "ss_r", "t1", "hb", "hT", "qk_q0", "qk_q1", "qk_k", "va", "qkr1", "qkr2", "ra", "rb", "rc", "rd",
                    "qkT", "ssq", "ssq_s", "ssq_r", "ssk", "ssk_s", "ssk_r", "cn_q", "cn_k", "kr", "krr1", "krr2", "cT", "qb_a", "qb_b",
                    "qb_r1", "qb_r2", "qr_a", "qr_b", "kb_n0", "kb_n1", "kb_r", "vb0", "vb1", "qbT", "kbT", "qk0", "qk1", "qk2", "vc",
                    "sq", "sq2", "ssh", "ssh_s", "ssh_r", "qkn", "qT_q", "qT_k"}
        identf, identb = _consts(p, es, nc, T)
        ncol = 1440 if layer == 0 else 1536
        win = sb("win", [128, 8, ncol], BF16)
        G1 = sb("G1", [128, 3, 1024], F32)
        S1 = sb("S1", [128, 3, 1024], F32)
        xt = [sb("xt%d" % i, [128, 1024], F32) for i in range(2)]
        rp = [sb("rp%d" % i, [128, 128], F32) for i in range(3)]
        junk = dsb("junk", [128, 1024], BF16)
        ss = dsb("ss", [128, 48], F32)
        t1 = dsb("t1", [128, 1024], F32)
        hb = dsb("hb", [128, 1024], BF16)
        hT = dsb("hT", [128, 8, 128], BF16)
        ptr = pm("ptr", [128, 8, 128], BF16)
        pp = [pm("pp%d" % i, [128, 512], F32) for i in range(4)]
        ptq = pm("ptq", [128, 8, 128], BF16)
        ptk = pm("ptk", [128, 8, 128], BF16)
        wname = "ab_w_in" if layer == 0 else "c_w_in"
        p.sc.dma("pool", lambda e: e.dma_start(out=win[:], in_=T[wname].rearrange("(k p) n -> p k n", p=128)), (), ["win"])
        _load_mod(p, S1, T, layer, 0, "S1")
        _load_mod(p, G1, T, layer, 1, "G1")
        if layer == 0:
            wqb = sb("wqb", [128, 3, 768], BF16)
            wkvb = sb("wkvb", [128, 2, 1024], BF16)
            gq = sb("gq", [128, 384], F32)
            gkv = sb("gkv", [128, 256], F32)
            p.sc.dma("pool", lambda e: e.dma_start(out=wqb[:], in_=T["mla_wq_b"].rearrange("(k p) n -> p k n", p=128)), (), ["wqb"])
            p.sc.dma("pool", lambda e: e.dma_start(out=wkvb[:], in_=T["mla_wkv_b"].rearrange("(k p) n -> p k n", p=128)), (), ["wkvb"])
            p.dma("sp", gq[:], T["mla_q_norm_g"].partition_broadcast(128), [], ["gq"])
            p.dma("sp", gkv[:], T["mla_kv_norm_g"].partition_broadcast(128), [], ["gkv"])
            qk = dsb("qk", [128, 10, 64], F32)
            qkr = dsb("qkr", [128, 10, 64], BF16)
            rtm = [dsb("rtm%d" % i, [128, 10, 32], F32) for i in range(4)]
            va = dsb("va", [128, 2, 65], BF16)
            qkT = dsb("qkT", [128, 5, 128], BF16)
            ssq = dsb("ssq", [128, 48], F32)
            ssk = dsb("ssk", [128, 48], F32)
            cn = dsb("cn", [128, 640], BF16)
            kr = dsb("kr", [128, 32], F32)
            krr = dsb("krr", [128, 32], BF16)
            cT = dsb("cT", [128, 5, 128], BF16)
            qb = dsb("qb", [128, 8, 96], BF16)
            qr = dsb("qr", [128, 8, 32], F32)
            kb = dsb("kb", [128, 8, 96], BF16)
            vb = dsb("vb", [128, 8, 65], BF16)
            qbT = dsb("qbT", [96, 8, 128], BF16)
            kbT = dsb("kbT", [96, 8, 128], BF16)
            for _i in range(2):
                p.setpar(_i)
                p.memset("pool", va[:], 1.0, ["va"])
                p.memset("pool", vb[:], 1.0, ["vb0", "vb1"])
            p.setpar(0)
            zt = sb("zt", [128, 4096], BF16)
            p.memset("dve", zt[:], 0.0, ["zt"])
            xsv = T["XS"].rearrange("(c p r) d -> c p (r d)", p=128, r=4)
            for c in range(NSLOT // 512):
                p.sc.dma("pool", lambda e_, c=c: e_.dma_start(out=xsv[c], in_=zt[:]), ["zt"], ["XSz%d" % (c % 4)])
        else:
            gqk = sb("gqk", [128, 10, 128], F32)
            gtmp = sb("gtmp", [128, 2, 128], F32)
            p.dma("sp", gtmp[:, 0, :], T["c_q_norm_g"].partition_broadcast(128), [], ["gtmp"])
            p.dma("sp", gtmp[:, 1, :], T["c_k_norm_g"].partition_broadcast(128), [], ["gtmp"])
            p.act(gqk[:, 0:8, :], gtmp[:, 0:1, :].to_broadcast([128, 8, 128]), AF.Copy, ["gtmp"], ["gqk"], scale=128.0 ** -0.5)
            p.cp("dve", gqk[:, 8:10, :], gtmp[:, 1:2, :].to_broadcast([128, 2, 128]), ["gtmp"], ["gqk"])
            qk = dsb("qk", [128, 10, 128], F32)
            sq = dsb("sq", [128, 10, 128], F32)
            ssh = dsb("ssh", [128, 30], F32)
            qkn = dsb("qkn", [128, 10, 128], F32)
            qkr = dsb("qkr", [128, 10, 128], BF16)
            rtm = [dsb("rtm%d" % i, [128, 10, 64], F32) for i in range(4)]
            vc = dsb("vc", [128, 2, 129], BF16)
            qT = dsb("qT", [128, 10, 128], BF16)
            for _i in range(2):
                p.setpar(_i)
                p.memset("pool", vc[:], 1.0, ["vc"])
            p.setpar(0)

        tiles = [(b, t) for b in range(NBC) for t in range(NT)]

        def src_of(b, t):
            if layer == 0:
                if t < 16:
                    return T["x"][b, t * 128:(t + 1) * 128, :]
                return T["ctx"][b, (t - 16) * 128:(t - 15) * 128, :]
            return T["XR"][b, t * 128:(t + 1) * 128, :]

        rope_t = T["ROPE0"] if layer == 0 else T["ROPE1"]
        rw = 96 if layer == 0 else 128

        def load(it):
            b, t = tiles[it]
            p.dma("sp", xt[it % 2][:], src_of(b, t), [], ["xt%d" % (it % 2)])
            if t < 16:
                p.dma("sp", rp[it % 3][:, 0:rw], rope_t[t * 128:(t + 1) * 128, :], [], ["rp%d" % (it % 3)])

        load(0)
        for it, (b, t) in enumerate(tiles):
            if PIPE and layer == 1:
                p.sc.key = (it, 0)
            if it + 1 < len(tiles):
                load(it + 1)
            p.setpar(it)
            lat = t < 16
            j = b if lat else 2
            x = xt[it % 2]
            xn = "xt%d" % (it % 2)
            r_ = rp[it % 3]
            rn = "rp%d" % (it % 3)
            rows = slice(t * 128, (t + 1) * 128)
            p.act(junk[:], x[:], AF.Square, [xn], ["junk", "ss"], accum=ss[:, 0:1])
            p.rstd(ss, 1, "ss", 1024)
            p.stt(t1[:], x[:], ss[:, 32:33], G1[:, j, :], ALU.mult, ALU.mult, [xn, "ss_r", "G1"], ["t1"])
            p.tt("pool", hb[:], t1[:], S1[:, j, :], ALU.add, ["t1", "S1"], ["hb"])
            for k in range(8):
                p.tr(ptr[:, k, :], hb[:, k * 128:(k + 1) * 128], identb[:], ["hb", "identb"], ["ptr"])
            p.cp("act", hT[:], ptr[:], ["ptr"], ["hT"])
            if layer == 0:
                chunks = [(0, 512), (512, 768), (768, 1152), (1152, 1440)]
                for ci, (c0, c1) in enumerate(chunks):
                    for k in range(8):
                        p.mm(pp[ci][:, 0:c1 - c0], hT[:, k, :], win[:, k, c0:c1], k == 0, k == 7, ["hT", "win"], ["pp%d" % ci])
                for g in range(2):
                    p.act(qk[:, 0:8, :].rearrange("p (j g) d -> p j g d", g=2)[:, :, g, :],
                          pp[0][:, g * 256:(g + 1) * 256].rearrange("p (j d) -> p j d", j=4), AF.Copy, ["pp0"], ["qk_q%d" % g], scale=0.125)
                p.cp("dve", qk[:, 8:10, :], pp[1][:, 0:128].rearrange("p (h d) -> p h d", h=2), ["pp1"], ["qk_k"])
                p.cp("dve", va[:, :, 0:64], pp[1][:, 128:256].rearrange("p (h d) -> p h d", h=2), ["pp1"], ["va"])
                if lat:
                    bc = lambda a: a.unsqueeze(1).to_broadcast([128, 10, 32])
                    _rope(p, qk[:, :, 0:32], qk[:, :, 32:64], bc(r_[:, 0:32]), bc(r_[:, 32:64]),
                          qkr[:, :, 0:32], qkr[:, :, 32:64], [m[:] for m in rtm], ["qk_q0", "qk_q1", "qk_k", rn], "qkr1", "qkr2")
                else:
                    p.cp("dve", qkr[:], qk[:], ["qk_q0", "qk_q1", "qk_k"], ["qkr1", "qkr2"])
                qkr2d = qkr[:].rearrange("p h d -> p (h d)")
                for c in range(5):
                    p.tr(ptr[:, c, :], qkr2d[:, c * 128:(c + 1) * 128], identb[:], ["qkr1", "qkr2", "identb"], ["ptr"])
                p.cp("act", qkT[:], ptr[:, 0:5, :], ["ptr"], ["qkT"])
                p.dma("sp", T["QAT"][b][:, :, rows], qkT[:, 0:4, :], ["qkT"], ["QAT"])
                p.dma("sp", T["KAT"][b][:, rows], qkT[:, 4, :], ["qkT"], ["KAT"])
                p.dma("sp", T["VA1"][b][rows], va[:], ["va"], ["VA1"])
                p.act(junk[:, 0:384], pp[2][:, 0:384], AF.Square, ["pp2"], ["junk", "ssq"], accum=ssq[:, 0:1])
                p.rstd(ssq, 1, "ssq", 384)
                p.act(junk[:, 0:256], pp[3][:, 0:256], AF.Square, ["pp3"], ["junk", "ssk"], accum=ssk[:, 0:1])
                p.rstd(ssk, 1, "ssk", 256)
                p.stt(cn[:, 0:384], pp[2][:, 0:384], ssq[:, 32:33], gq[:], ALU.mult, ALU.mult, ["pp2", "ssq_r", "gq"], ["cn_q"])
                p.stt(cn[:, 384:640], pp[3][:, 0:256], ssk[:, 32:33], gkv[:], ALU.mult, ALU.mult, ["pp3", "ssk_r", "gkv"], ["cn_k"])
                p.cp("act", kr[:], pp[3][:, 256:288], ["pp3"], ["kr"])
                if lat:
                    _rope(p, kr[:, 0:16], kr[:, 16:32], r_[:, 64:80], r_[:, 80:96], krr[:, 0:16], krr[:, 16:32],
                          [m[:, 0, 0:16] for m in rtm], ["kr", rn], "krr1", "krr2")
                else:
                    p.cp("dve", krr[:], kr[:], ["kr"], ["krr1", "krr2"])
                for c in range(5):
                    p.tr(ptr[:, c, :], cn[:, c * 128:(c + 1) * 128], identb[:], ["cn_q", "cn_k", "identb"], ["ptr"])
                p.cp("act", cT[:], ptr[:, 0:5, :], ["ptr"], ["cT"])
                for ci, (c0, c1) in enumerate([(0, 480), (480, 768)]):
                    for k in range(3):
                        p.mm(pp[ci][:, 0:c1 - c0], cT[:, k, :], wqb[:, k, c0:c1], k == 0, k == 2, ["cT", "wqb"], ["pp%d" % ci])
                for ci in range(2):
                    for k in range(2):
                        p.mm(pp[2 + ci][:, 0:512], cT[:, 3 + k, :], wkvb[:, k, ci * 512:(ci + 1) * 512], k == 0, k == 1, ["cT", "wkvb"], ["pp%d" % (2 + ci)])
                s = 96.0 ** -0.5
                qv0 = pp[0][:, 0:480].rearrange("p (h d) -> p h d", h=5)
                qv1 = pp[1][:, 0:288].rearrange("p (h d) -> p h d", h=3)
                if lat:
                    p.act(qb[:, 0:5, 0:64], qv0[:, :, 0:64], AF.Copy, ["pp0"], ["qb_a"], scale=s)
                    p.act(qb[:, 5:8, 0:64], qv1[:, :, 0:64], AF.Copy, ["pp1"], ["qb_b"], scale=s)
                    p.act(qr[:, 0:5, :], qv0[:, :, 64:96], AF.Copy, ["pp0"], ["qr_a"], scale=s)
                    p.act(qr[:, 5:8, :], qv1[:, :, 64:96], AF.Copy, ["pp1"], ["qr_b"], scale=s)
                    bc8 = lambda a: a.unsqueeze(1).to_broadcast([128, 8, 16])
                    _rope(p, qr[:, :, 0:16], qr[:, :, 16:32], bc8(r_[:, 64:80]), bc8(r_[:, 80:96]),
                          qb[:, :, 64:80], qb[:, :, 80:96], [m[:, 0:8, 0:16] for m in rtm], ["qr_a", "qr_b", rn], "qb_r1", "qb_r2")
                else:
                    p.act(qb[:, 0:5, :], qv0, AF.Copy, ["pp0"], ["qb_a", "qb_r1"], scale=s)
                    p.act(qb[:, 5:8, :], qv1, AF.Copy, ["pp1"], ["qb_b", "qb_r2"], scale=s)
                for ci in range(2):
                    kvv = pp[2 + ci][:, 0:512].rearrange("p (h d) -> p h d", h=4)
                    p.cp("dve", kb[:, 4 * ci:4 * ci + 4, 0:64], kvv[:, :, 0:64], ["pp%d" % (2 + ci)], ["kb_n%d" % ci])
                    p.cp("act", vb[:, 4 * ci:4 * ci + 4, 0:64], kvv[:, :, 64:128], ["pp%d" % (2 + ci)], ["vb%d" % ci])
                p.cp("pool", kb[:, :, 64:96], krr[:].unsqueeze(1).to_broadcast([128, 8, 32]), ["krr1", "krr2"], ["kb_r"])
                for h in range(8):
                    p.tr(ptq[0:96, h, :], qb[:, h, :], identb[:], ["qb_a", "qb_b", "qb_r1", "qb_r2", "identb"], ["ptq"])
                for h in range(8):
                    p.tr(ptk[0:96, h, :], kb[:, h, :], identb[:], ["kb_n0", "kb_n1", "kb_r", "identb"], ["ptk"])
                p.cp("dve", qbT[:], ptq[0:96], ["ptq"], ["qbT"])
                p.cp("act", kbT[:], ptk[0:96], ["ptk"], ["kbT"])
                p.dma("sp", T["QBT"][b][:, :, rows], qbT[:], ["qbT"], ["QBT"])
                p.dma("sp", T["KBT"][b][:, :, rows], kbT[:], ["kbT"], ["KBT"])
                p.dma("sp", T["VB1"][b][rows], vb[:], ["vb0", "vb1"], ["VB1"])
            else:
                h0 = 0 if lat else 8
                if lat:
                    for ci in range(2):
                        for k in range(8):
                            p.mm(pp[ci][:], hT[:, k, :], win[:, k, ci * 512:(ci + 1) * 512], k == 0, k == 7, ["hT", "win"], ["pp%d" % ci])
                for k in range(8):
                    p.mm(pp[2][:], hT[:, k, :], win[:, k, 1024:1536], k == 0, k == 7, ["hT", "win"], ["pp2"])
                if lat:
                    p.cp("act", qk[:, 0:4, :], pp[0][:].rearrange("p (h d) -> p h d", h=4), ["pp0"], ["qk0"])
                    p.cp("act", qk[:, 4:8, :], pp[1][:].rearrange("p (h d) -> p h d", h=4), ["pp1"], ["qk1"])
                p.cp("act", qk[:, 8:10, :], pp[2][:, 0:256].rearrange("p (h d) -> p h d", h=2), ["pp2"], ["qk2"])
                p.cp("dve", vc[:, :, 0:128], pp[2][:, 256:512].rearrange("p (h d) -> p h d", h=2), ["pp2"], ["vc"])
                if PIPE:
                    p.sc.key = (it, 1)
                qkd = ["qk0", "qk1", "qk2"]
                nh = 10 - h0
                p.tt("pool", sq[:, h0:10, :], qk[:, h0:10, :], qk[:, h0:10, :], ALU.mult, qkd, ["sq"])
                p.sc.op("dve", lambda e, o_=ssh[:, h0:10], i_=sq[:, h0:10, :]: e.tensor_reduce(out=o_, in_=i_, axis=AX.X, op=ALU.add), ["sq"], ["ssh"])
                p.act(ssh[:, 10 + h0:20], ssh[:, h0:10], AF.Sqrt, ["ssh"], ["ssh_s"], scale=1.0 / 128, bias=p.eps_ap)
                p.rcp(ssh[:, 20 + h0:30], ssh[:, 10 + h0:20], ["ssh_s"], ["ssh_r"])
                p.tt("dve", sq[:, h0:10, :], qk[:, h0:10, :], ssh[:, 20 + h0:30].unsqueeze(2).to_broadcast([128, nh, 128]), ALU.mult, qkd + ["ssh_r", "sq"], ["sq2"])
                p.tt("pool", qkn[:, h0:10, :], sq[:, h0:10, :], gqk[:, h0:10, :], ALU.mult, ["sq2", "gqk"], ["qkn"])
                if lat:
                    bc = lambda a: a.unsqueeze(1).to_broadcast([128, 10, 64])
                    _rope(p, qkn[:, :, 0:64], qkn[:, :, 64:128], bc(r_[:, 0:64]), bc(r_[:, 64:128]),
                          qkr[:, :, 0:64], qkr[:, :, 64:128], [m[:] for m in rtm], ["qkn", rn], "qkr1", "qkr2")
                else:
                    p.cp("dve", qkr[:, 8:10, :], qkn[:, 8:10, :], ["qkn"], ["qkr1", "qkr2"])
                if lat:
                    for h in range(8):
                        p.tr(ptq[:, h, :], qkr[:, h, :], identb[:], ["qkr1", "qkr2", "identb"], ["ptq"])
                    p.cp("act", qT[:, 0:8, :], ptq[:], ["ptq"], ["qT_q"])
                    p.dma("sp", T["QCT"][b][:, :, rows], qT[:, 0:8, :], ["qT_q"], ["QCT"])
                for h in range(2):
                    p.tr(ptk[:, h, :], qkr[:, 8 + h, :], identb[:], ["qkr1", "qkr2", "identb"], ["ptk"])
                p.cp("dve", qT[:, 8:10, :], ptk[:, 0:2, :], ["ptk"], ["qT_k"])
                p.dma("sp", T["KCT"][b][:, :, rows], qT[:, 8:10, :], ["qT_k"], ["KCT"])
                p.dma("sp", T["VC1"][b][rows], vc[:], ["vc"], ["VC1"])
        p.sc.key = None
        p.sc.emit()


def phase_attn_a(nc, T):
    with nc.cleanup_on_exit(), ExitStack() as es:
        sb = lambda name, shape, dt: es.enter_context(nc.sbuf_tensor("u%d_" % _U[0] + name, shape, dt))
        pm = lambda name, shape, dt: es.enter_context(nc.psum_tensor("u%d_" % _U[0] + name, shape, dt))
        p = Ph(nc)
        qat = [sb("qat%d" % i, [128, 4, TOK], BF16) for i in range(2)]
        kat = [sb("kat%d" % i, [128, TOK], BF16) for i in range(2)]
        va1 = [sb("va1%d" % i, [128, NT, 2, 65], BF16) for i in range(2)]
        esink = sb("esink", [128, 8], F32)
        mk = sb("mk", [128, 2, 128], F32)
        mkb = sb("mkb", [128, 2, 128], BF16)
        pT = [sb("pT%d" % i, [128, 5, 512], BF16) for i in range(2)]
        den = sb("den", [128, 16], F32)
        osb = [sb("osb%d" % i, [128, 4, 64], BF16) for i in range(2)]
        psc = [pm("psc%d" % i, [128, 512], F32) for i in range(4)]
        po = [[pm("po%d_%d" % (i, h), [128, 2, 65], F32) for h in range(2)] for i in range(2)]
        p.dma("sp", esink[:], T["swa_sink"].partition_broadcast(128), [], ["esink"])
        p.act(esink[:], esink[:], AF.Exp, ["esink"], ["esink"])
        p.dma("sp", mk[:], T["MASKS"].rearrange("m p q -> p m q"), [], ["mk"])
        p.cp("dve", mkb[:], mk[:], ["mk"], ["masks"])
        zf = sb("zf", [128, 1024], F32)
        p.memset("pool", zf[:], 0.0, ["zf"])
        p.dma("sp", T["YS"][NSLOT:NSLOT + 128, :], zf[:], ["zf"], ["YSz"])
        cnt = [0, 0]
        for b in range(NBC):
            p.dma("sp", qat[b][:], T["QAT"][b], [], ["qat%d" % b])
            p.dma("sp", kat[b][:], T["KAT"][b], [], ["kat%d" % b])
            p.dma("sp", va1[b][:], T["VA1"][b].rearrange("(t p) g d -> p t g d", p=128), [], ["va1%d" % b])
        pend = None
        for b in range(NBC):
            for qi in range(NT):
                for g in range(2):
                    if qi < 16:
                        lk = [k for k in (qi - 1, qi, qi + 1) if 0 <= k < 16]
                        kts = lk + [16, 17]
                        masks = [mkb[:, 0, :] if k == qi - 1 else (mkb[:, 1, :] if k == qi + 1 else None) for k in lk] + [None, None]
                    else:
                        kts = [16, 17]
                        masks = None
                    o_ = osb[cnt[0] % 2]
                    on = "osb%d" % (cnt[0] % 2)
                    pvf = _attn_core_named(p, kat[b][g * 64:(g + 1) * 64, :], "kat%d" % b,
                                           qat[b][g * 64:(g + 1) * 64, :, qi * 128:(qi + 1) * 128], "qat%d" % b,
                                           va1[b], "va1%d" % b, g, kts, 512, 64, masks, psc, pT, po, den, o_, on,
                                           esink[:, 4 * g:4 * g + 4], cnt)
                    if pend is not None:
                        pend()

                    def pend(pvf=pvf, b=b, qi=qi, g=g, o_=o_, on=on):
                        pvf()
                        p.dma("sp", T["OATT"][b][qi * 128:(qi + 1) * 128, g * 256:(g + 1) * 256],
                              o_[:].rearrange("p j d -> p (j d)"), [on + "_0", on + "_1"], ["OATT"])
        pend()
        p.sc.emit()


def _attn_core_named(p, kT, kname, qv, qname, v1, vname, hv, kts, nq, dv, masks, psc, pT, po, den, osb, osbn, sink_ap, cnt):
    nk = len(kts)
    nj = nq // 128
    base = cnt[0]
    cnt[0] += 1
    pTt = pT[base % 2]
    pTn = "pT%d" % (base % 2)
    for n, kt in enumerate(kts):
        ps = psc[cnt[1] % 4]
        psn = "psc%d" % (cnt[1] % 4)
        cnt[1] += 1
        p.mm(ps[:, 0:nq], kT[:, kt * 128:(kt + 1) * 128], qv, True, True, [kname, qname], [psn])
        p.act(pTt[:, n, 0:nq], ps[:, 0:nq], AF.Exp, [psn], [pTn + "_%d" % n])
        if masks is not None and masks[n] is not None:
            v_ = pTt[:, n, 0:nq].rearrange("p (j q) -> p j q", q=128)
            p.tt("pool", v_, v_, masks[n].unsqueeze(1).to_broadcast([128, nj, 128]), ALU.mult, [pTn + "_%d" % n, "masks"], [pTn + "_%d" % n])
    return lambda: _attn_pv(p, base, pTt, pTn, v1, vname, hv, kts, nj, dv, po, den, osb, osbn, sink_ap)


def _attn_pv(p, base, pTt, pTn, v1, vname, hv, kts, nj, dv, po, den, osb, osbn, sink_ap):
    nk = len(kts)
    pset = po[base % 2]
    pon = "po%d" % (base % 2)
    dnb = "den%d" % (base % 2)
    dd = den[:, (base % 2) * 8:(base % 2) * 8 + 8]
    for j in range(nj):
        pj = pset[j // 2][:, j % 2, 0:dv + 1]
        for n, kt in enumerate(kts):
            p.mm(pj, pTt[:, n, j * 128:(j + 1) * 128], v1[:, kt, hv, 0:dv + 1], n == 0, n == nk - 1,
                 [pTn + "_%d" % n, vname], [pon + "_%d" % (j // 2)])
    for half in range((nj + 1) // 2):
        j0 = half * 2
        nn = min(2, nj - j0)
        pv = pset[half]
        dn = dnb + "_%d" % half
        if sink_ap is not None:
            p.tt("dve", dd[:, j0:j0 + nn], pv[:, 0:nn, dv], sink_ap[:, j0:j0 + nn], ALU.add, [pon + "_%d" % half, "esink"], [dn])
            p.rcp(dd[:, 4 + j0:4 + j0 + nn], dd[:, j0:j0 + nn], [dn], [dn + "r"])
        else:
            p.rcp(dd[:, 4 + j0:4 + j0 + nn], pv[:, 0:nn, dv], [pon + "_%d" % half], [dn + "r"])
        p.tt("dve", osb[:, j0:j0 + nn, 0:dv], pv[:, 0:nn, 0:dv], dd[:, 4 + j0:4 + j0 + nn].unsqueeze(2).to_broadcast([128, nn, dv]),
             ALU.mult, [pon + "_%d" % half, dn + "r"], [osbn + "_%d" % half])


def phase_attn_dense(nc, T, layer):
    with nc.cleanup_on_exit(), ExitStack() as es:
        sb = lambda name, shape, dt: es.enter_context(nc.sbuf_tensor("u%d_" % _U[0] + name, shape, dt))
        pm = lambda name, shape, dt: es.enter_context(nc.psum_tensor("u%d_" % _U[0] + name, shape, dt))
        p = Ph(nc)
        if layer == 0:
            hd, dv, nkv, nqc = 96, 64, 8, TOK
        else:
            hd, dv, nkv, nqc = 128, 128, 2, SEQ
        qT = [sb("qT%d" % i, [hd, nqc], BF16) for i in range(2)]
        kT = [sb("kT%d" % i, [hd, TOK], BF16) for i in range(2)]
        v1 = [sb("v1%d" % i, [128, NT, nkv, dv + 1], BF16) for i in range(2)]
        pT = [sb("pT%d" % i, [128, NT, 512], BF16) for i in range(2)]
        den = sb("den", [128, 16], F32)
        osb = [sb("osb%d" % i, [128, 4, dv], BF16) for i in range(2)]
        psc = [pm("psc%d" % i, [128, 512], F32) for i in range(4)]
        po = [[pm("po%d_%d" % (i, h), [128, 2, dv + 1], F32) for h in range(2)] for i in range(2)]
        cnt = [0, 0]
        hi = 0
        vsrc = T["VB1"] if layer == 0 else T["VC1"]
        for b in range(NBC):
            p.dma("sp", v1[b][:], vsrc[b].rearrange("(t p) g d -> p t g d", p=128), [], ["v1%d" % b])
        pend = None
        for b in range(NBC):
            for h in range(8):
                q_ = qT[hi % 2]
                qn = "qT%d" % (hi % 2)
                if layer == 0:
                    k_ = kT[hi % 2]
                    kn = "kT%d" % (hi % 2)
                    p.dma("sp", q_[:], T["QBT"][b][:, h, :], [], [qn])
                    p.dma("sp", k_[:], T["KBT"][b][:, h, :], [], [kn])
                    hv = h
                    col0 = 512 + h * 64
                else:
                    g = h // 4
                    k_ = kT[(b * 2 + g) % 2]
                    kn = "kT%d" % ((b * 2 + g) % 2)
                    p.dma("sp", q_[:], T["QCT"][b][:, h, :], [], [qn])
                    if h % 4 == 0:
                        p.dma("sp", k_[:], T["KCT"][b][:, g, :], [], [kn])
                    hv = g
                    col0 = h * 128
                hi += 1
                chunks = [(c * 512, 512, list(range(NT))) for c in range(4)]
                if layer == 0:
                    chunks.append((SEQ, 256, [16, 17]))
                for (q0, nq, kts) in chunks:
                    o_ = osb[cnt[0] % 2]
                    on = "osb%d" % (cnt[0] % 2)
                    pvf = _attn_core_named(p, k_[:], kn, q_[:, q0:q0 + nq], qn, v1[b], "v1%d" % b, hv, kts, nq, dv, None,
                                           psc, pT, po, den, o_, on, None, cnt)
                    if pend is not None:
                        pend()

                    def pend(pvf=pvf, b=b, q0=q0, nq=nq, col0=col0, o_=o_, on=on):
                        pvf()
                        nj = nq // 128
                        p.dma("sp", T["OATT"][b][q0:q0 + nq, col0:col0 + dv].rearrange("(j p) d -> p j d", p=128),
                              o_[:, 0:nj, :], [on + "_0", on + "_1"], ["OATT"])
        pend()
        p.sc.emit()


def phase_o(nc, T, layer):
    with nc.cleanup_on_exit(), ExitStack() as es:
        sb = lambda name, shape, dt: es.enter_context(nc.sbuf_tensor("u%d_" % _U[0] + name, shape, dt))
        pm = lambda name, shape, dt: es.enter_context(nc.psum_tensor("u%d_" % _U[0] + name, shape, dt))
        p = Ph(nc)
        dsb = lambda name, shape, dt: Dbl(p, sb(name + "A", shape, dt), sb(name + "B", shape, dt))
        p.sc.dbl = {"oT", "t1_0", "t1_1", "xn_0", "xn_1", "junk", "ss", "ss_s", "ss_r", "h2", "h2T0", "h2T1", "lg", "v8", "sm0", "sm1", "sm2",
                    "ex", "sel", "pos", "ovf", "posb", "pos3", "ohk", "j32", "slotf0", "slotf1", "slotf2", "slotf3", "slotg"}
        identf, identb = _consts(p, es, nc, T)
        wout = sb("wout", [128, 8, 1024], BF16)
        g1 = sb("g1", [128, 3, 1024], F32)
        G2 = sb("G2", [128, 3, 1024], F32)
        S2 = sb("S2", [128, 3, 1024], F32)
        rw = sb("rw", [128, 8, 32], F32)
        rb = sb("rb", [128, 32], F32)
        ecap = sb("ecap", [128, 32], F32)
        trif = sb("trif", [128, 2, 128], F32)
        trib = sb("trib", [128, 2, 128], BF16)
        cntt = sb("cntt", [128, 32], F32)
        cnti = sb("cnti", [1, 32], I32)
        ot = [sb("ot%d" % i, [128, 1024], BF16) for i in range(2)]
        xt = [sb("xt%d" % i, [128, 1024], F32) for i in range(2)]
        oT = dsb("oT", [128, 8, 128], BF16)
        t1 = dsb("t1", [128, 1024], F32)
        xn_ = dsb("xn", [128, 1024], F32)
        junk = dsb("junk", [128, 1024], BF16)
        ss = dsb("ss", [128, 48], F32)
        h2 = dsb("h2", [128, 1024], F32)
        h2b = [sb("h2b%d" % i, [128, 1024], BF16) for i in range(2)]
        h2T = dsb("h2T", [128, 8, 128], F32)
        lg = dsb("lg", [128, 32], F32)
        v8 = dsb("v8", [128, 8], F32)
        sm = dsb("sm", [128, 8], F32)
        ex = dsb("ex", [128, 4], F32)
        gate = [sb("gate%d" % i, [128, 4], F32) for i in range(2)]
        sel = dsb("sel", [128, 32], BF16)
        pos = dsb("pos", [128, 32], F32)
        ovf = dsb("ovf", [128, 32], F32)
        posb = dsb("posb", [128, 32], F32)
        posc = dsb("posc", [128, 32], F32)
        ohk = dsb("ohk", [128, 32], F32)
        j32 = dsb("j32", [128, 32], F32)
        slotf = dsb("slotf", [128, 8], F32)
        sls = [sb("sls%d" % i, [128, 4], I32) for i in range(2)]
        slg = [sb("slg%d" % i, [128, 4], I32) for i in range(2)]
        ptr = pm("ptr", [128, 8, 128], BF16)
        pp = [pm("pp%d" % i, [128, 512], F32) for i in range(2)]
        ptf = pm("ptf", [128, 4, 128], F32)
        psl = pm("psl", [128, 32], F32)
        psr = pm("psr", [128, 2, 32], F32)
        wname = "ab_w_out" if layer == 0 else "c_w_out"
        p.sc.dma("pool", lambda e: e.dma_start(out=wout[:], in_=T[wname].rearrange("(k p) n -> p k n", p=128)), (), ["wout"])
        _load_mod(p, g1, T, layer, 2, "g1")
        _load_mod(p, S2, T, layer, 3, "S2")
        _load_mod(p, G2, T, layer, 4, "G2")
        p.dma("sp", rw[:], T["router_w"][layer].rearrange("(k p) e -> p k e", p=128), [], ["rw"])
        p.dma("sp", rb[:], T["router_b"][layer].partition_broadcast(128), [], ["rb"])
        p.dma("sp", ecap[:], T["ECAP"], [], ["ecap"])
        p.dma("sp", trif[:], T["TRI"].rearrange("m p q -> p m q"), [], ["trif"])
        p.cp("dve", trib[:], trif[:], ["trif"], ["trib"])
        p.memset("pool", cntt[:], 0.0, ["cnt"])
        ntl = NT if layer == 0 else 16
        tiles = [(b, t) for b in range(NBC) for t in range(ntl)]

        def xsrc(b, t):
            if layer == 0:
                if t < 16:
                    return T["x"][b, t * 128:(t + 1) * 128, :]
                return T["ctx"][b, (t - 16) * 128:(t - 15) * 128, :]
            return T["XR"][b, t * 128:(t + 1) * 128, :]

        def load(it):
            b, t = tiles[it]
            p.dma("sp", ot[it % 2][:], T["OATT"][b][t * 128:(t + 1) * 128, :], [], ["ot%d" % (it % 2)])
            p.dma("sp", xt[it % 2][:], xsrc(b, t), [], ["xt%d" % (it % 2)])

        load(0)
        for it, (b, t) in enumerate(tiles):
            p.sc.key = (it, 0) if PIPE else None
            if it + 1 < len(tiles):
                load(it + 1)
            p.setpar(it)
            j = b if t < 16 else 2
            rows = slice(t * 128, (t + 1) * 128)
            o_ = ot[it % 2]
            on = "ot%d" % (it % 2)
            x = xt[it % 2]
            xn = "xt%d" % (it % 2)
            hb_ = h2b[it % 2]
            hbn = "h2b%d" % (it % 2)
            ga = gate[it % 2]
            gan = "gate%d" % (it % 2)
            ss_ = sls[it % 2]
            ssn = "sls%d" % (it % 2)
            sg_ = slg[it % 2]
            sgn = "slg%d" % (it % 2)
            for k in range(8):
                p.tr(ptr[:, k, :], o_[:, k * 128:(k + 1) * 128], identb[:], [on, "identb"], ["ptr"])
            p.cp("act", oT[:], ptr[:], ["ptr"], ["oT"])
            for hf in range(2):
                for k in range(8):
                    p.mm(pp[hf][:], oT[:, k, :], wout[:, k, hf * 512:(hf + 1) * 512], k == 0, k == 7, ["oT", "wout"], ["pp%d" % hf])
            for hf in range(2):
                sl = slice(hf * 512, (hf + 1) * 512)
                p.tt("dve", t1[:, sl], pp[hf][:], g1[:, j, sl], ALU.mult, ["pp%d" % hf, "g1"], ["t1_%d" % hf])
                p.tt("pool", xn_[:, sl], t1[:, sl], x[:, sl], ALU.add, ["t1_%d" % hf, xn], ["xn_%d" % hf])
            p.dma("sp", T["XR"][b, rows, :], xn_[:], ["xn_0", "xn_1"], ["XR"])
            p.sc.key = (it, 1) if PIPE else None
            p.act(junk[:], xn_[:], AF.Square, ["xn_0", "xn_1"], ["junk", "ss"], accum=ss[:, 0:1])
            p.rstd(ss, 1, "ss", 1024)
            p.stt(t1[:], xn_[:], ss[:, 32:33], G2[:, j, :], ALU.mult, ALU.mult, ["xn_0", "xn_1", "ss_r", "G2", "t1_0", "t1_1"], ["t1_0", "t1_1"])
            p.tt("pool", h2[:], t1[:], S2[:, j, :], ALU.add, ["t1_0", "t1_1", "S2"], ["h2"])
            p.cp("act", hb_[:], h2[:], ["h2"], [hbn])
            for half in range(2):
                for k in range(4):
                    kk = half * 4 + k
                    p.tr(ptf[:, k, :], h2[:, kk * 128:(kk + 1) * 128], identf[:], ["h2", "identf"], ["ptf"])
                p.cp("dve", h2T[:, half * 4:half * 4 + 4, :], ptf[:], ["ptf"], ["h2T%d" % half])
            for k in range(8):
                p.mm(psl[:], h2T[:, k, :], rw[:, k, :], k == 0, k == 7, ["h2T0", "h2T1", "rw"], ["psl"])
            p.tt("dve", lg[:], psl[:], rb[:], ALU.add, ["psl", "rb"], ["lg"])
            p.sc.op("dve", lambda e, o_=v8[:], i_=lg[:]: e.max(out=o_, in_=i_), ["lg"], ["v8"])
            p.ts("dve", sm[:, 0:1], v8[:, 0:1], -1.0, None, ALU.mult, None, ["v8"], ["sm0"])
            p.act(ex[:], v8[:, 0:4], AF.Exp, ["v8", "sm0"], ["ex", "sm1"], bias=sm[:, 0:1], accum=sm[:, 1:2])
            p.rcp(sm[:, 2:3], sm[:, 1:2], ["sm1"], ["sm2"])
            p.ts("dve", ga[:], ex[:], sm[:, 2:3], None, ALU.mult, None, ["ex", "sm2"], [gan])
            p.ts("dve", sel[:], lg[:], v8[:, 3:4], None, ALU.is_ge, None, ["lg", "v8"], ["sel"])
            p.mm(psr[:, 0, :], trib[:, 0, :], sel[:], True, True, ["trib", "sel"], ["psr"])
            p.mm(psr[:, 1, :], trib[:, 1, :], sel[:], True, True, ["trib", "sel"], ["psr"])
            p.tt("dve", pos[:], psr[:, 0, :], cntt[:], ALU.add, ["psr", "cnt"], ["pos"])
            p.tt("dve", cntt[:], psr[:, 1, :], cntt[:], ALU.add, ["psr", "cnt"], ["cnt"])
            p.ts("dve", ovf[:], pos[:], float(CAP), BIG, ALU.is_ge, ALU.mult, ["pos"], ["ovf"])
            p.tt("dve", posb[:], pos[:], ecap[:], ALU.add, ["pos", "ecap"], ["posb"])
            p.tt("dve", posc[:], posb[:], ovf[:], ALU.add, ["posb", "ovf"], ["pos3"])
            for k in range(4):
                p.ts("dve", ohk[:], lg[:], v8[:, k:k + 1], None, ALU.is_equal, None, ["lg", "v8"], ["ohk"])
                p.stt(j32[:], ohk[:], 1.0, posc[:], ALU.mult, ALU.mult, ["ohk", "pos3"], ["j32", "slotf%d" % k], accum=slotf[:, k:k + 1])
            sfr = ["slotf%d" % k for k in range(4)]
            p.cp("dve", ss_[:], slotf[:, 0:4], sfr, [ssn])
            p.ts("dve", slotf[:, 4:8], slotf[:, 0:4], float(NSLOT), None, ALU.min, None, sfr, ["slotg"])
            p.cp("dve", sg_[:], slotf[:, 4:8], ["slotg"], [sgn])
            for k in range(4):
                p.sc.dma("pool", lambda e, k=k, ss_=ss_, hb_=hb_: e.indirect_dma_start(
                    out=T["XS"], out_offset=bass.IndirectOffsetOnAxis(ap=ss_[:, k:k + 1], axis=0),
                    in_=hb_[:], in_offset=None, bounds_check=p.reg(e, NSLOT - 1), oob_is_err=False), [hbn, ssn], ["XS%d" % k])
            p.dma("sp", T["SLOT"][b, rows, :], sg_[:], [sgn], ["SLOT"])
            p.dma("sp", T["GATE"][b, rows, :], ga[:], [gan], ["GATE"])
        p.sc.key = None
        p.dma("sp", T["CNT"][layer], cntt[:], ["cnt"], ["CNTd"])
        p.cp("dve", cnti[:], cntt[0:1, :], ["cnt"], ["cnti"])
        p.dma("sp", T["CNTI"][layer], cnti[:], ["cnti"], ["CNTId"])
        p.sc.emit()


def phase_e(nc, T, layer):
    with nc.cleanup_on_exit(), ExitStack() as es:
        sb = lambda name, shape, dt: es.enter_context(nc.sbuf_tensor("u%d_" % _U[0] + name, shape, dt))
        pm = lambda name, shape, dt: es.enter_context(nc.psum_tensor("u%d_" % _U[0] + name, shape, dt))
        p = Ph(nc)
        identf, identb = _consts(p, es, nc, T)
        win = [sb("win%d" % i, [128, 8, 2048], BF16) for i in range(2)]
        wout = [sb("wout%d" % i, [128, 8, 1024], BF16) for i in range(2)]
        bout = [sb("bout%d" % i, [128, 1024], F32) for i in range(2)]
        bsb = sb("bsb", [32, 2048], F32)
        bias = sb("bias", [128, 16, 32], F32)
        xg = [sb("xg%d" % i, [128, 1024], BF16) for i in range(2)]
        xT = [sb("xT%d" % i, [128, 8, 512], BF16) for i in range(2)]
        aT = [sb("aT%d" % i, [128, 8, 512], BF16) for i in range(2)]
        glu = sb("glu", [128, 512], F32)
        gluD = [glu, sb("glu1", [128, 512], F32)]
        sigD = [sb("sigD%d" % i, [128, 512], F32) for i in range(2)]
        linD = [sb("linD%d" % i, [128, 512], F32) for i in range(2)]
        sig = sb("sig", [128, 512], F32)
        lin = sb("lin", [128, 512], F32)
        gs = sb("gs", [128, 512], F32)
        lin2 = sb("lin2", [128, 512], F32)
        ysb = [sb("ysb%d" % i, [128, 1024], F32) for i in range(2)]
        ptr = pm("ptr", [128, 8, 128], BF16)
        pg = [pm("pg%d" % i, [128, 512], F32) for i in range(2)]
        pl = [pm("pl%d" % i, [128, 512], F32) for i in range(2)]
        py = [pm("py%d" % i, [128, 512], F32) for i in range(2)]
        ptb = pm("ptb", [128, 16, 32], F32)
        p.dma("sp", bsb[:], T["moe_b_in"][layer], [], ["bsb"])
        bv = bsb[:].rearrange("e (fc p two) -> e fc two p", p=128, two=2)
        for two in range(2):
            for fc in range(8):
                p.tr(ptb[:, two * 8 + fc, :], bv[:, fc, two, :], identf[0:32, 0:32], ["bsb", "identf"], ["ptb"])
        p.cp("dve", bias[:], ptb[:], ["ptb"], ["bias"])
        p.ts("dve", bias[:, 8:16, :], bias[:, 8:16, :], 1.0, None, ALU.add, None, ["bias"], ["bias"])

        def loadw(e):
            w_ = win[e % 2]
            o_ = wout[e % 2]
            src = T["moe_w_in"][layer, e].rearrange("(k p) n -> p k n", p=128)
            for q in range(4):
                p.sc.dma("pool", lambda en, w_=w_, src=src, q=q: en.dma_start(out=w_[:, 2 * q:2 * q + 2, :], in_=src[:, 2 * q:2 * q + 2, :]), (), ["win%d_%d" % (e % 2, q)])
            src2 = T["moe_w_out"][layer, e].rearrange("(k p) n -> p k n", p=128)
            for q in range(2):
                p.sc.dma("pool", lambda en, o_=o_, src2=src2, q=q: en.dma_start(out=o_[:, 4 * q:4 * q + 4, :], in_=src2[:, 4 * q:4 * q + 4, :]), (), ["wout%d_%d" % (e % 2, q)])
            p.dma("sp", bout[e % 2][:], T["moe_b_out"][layer, e].partition_broadcast(128), [], ["bout%d" % (e % 2)])

        cs = sb("cs", [1, NE], I32)
        p.sc.cnt_ap = lambda key: cs[0:1, key:key + 1]
        p.sc.cnt_dep = p.sc.dma("sp", lambda e_: e_.dma_start(out=cs[:], in_=T["CNTI"][layer]), (), ["cs"])
        for i in range(2):
            p.memset("dve", xT[i][:], 0.0, ["xT%d_%d" % (i, tt) for tt in range(4)])
        loadw(0)
        if os.environ.get("KPROBE") == "noweights":
            loadw(1)
            loadw = lambda e: None
        groups = [(e, grp) for e in range(NE) for grp in range(CAP // 512)]
        st = {"ti": 0, "fi": 0, "yi": 0}

        def stage_l(g):
            e, grp = groups[g]
            base = grp * 512
            xT_ = xT[g % 2]
            xTn = "xT%d" % (g % 2)
            for tt in range(4):
                p.sc.cur_cond = (e, base + tt * 128)
                s0 = e * CAP + base + tt * 128
                x_ = xg[st["ti"] % 2]
                xn = "xg%d" % (st["ti"] % 2)
                st["ti"] += 1
                p.dma("sp", x_[:], T["XS"][s0:s0 + 128, :], [], [xn])
                for k in range(8):
                    p.tr(ptr[:, k, :], x_[:, k * 128:(k + 1) * 128], identb[:], [xn, "identb"], ["ptr"])
                p.cp("dve", xT_[:, :, tt * 128:(tt + 1) * 128], ptr[:], ["ptr"], [xTn + "_%d" % tt])
                p.sc.cur_cond = None

        def stage_c(g):
            e, grp = groups[g]
            base = grp * 512
            xT_ = xT[g % 2]
            xTn = "xT%d" % (g % 2)
            aT_ = aT[g % 2]
            aTn = "aT%d" % (g % 2)
            w_ = win[e % 2]
            wv = w_[:].rearrange("p k (f two) -> p k f two", two=2)
            wr = ["win%d_%d" % (e % 2, q) for q in range(4)]
            if grp < 2:
                variants = [(base, 0, 256, (0,)), (base + 256, 256, 512, (1,))]
            else:
                variants = [(base, 0, 512, (0, 1))]
            for thr, c0, c1, hws in variants:
                fin = None
                p.sc.cur_cond = (e, thr)
                c_ = slice(c0, c1)
                w = c1 - c0
                xr = [xTn + "_%d" % tt for tt in range(c0 // 128, c1 // 128)]
                for fc in range(8):
                    fi = st["fi"]
                    st["fi"] += 1
                    pg_ = pg[fi % 2]
                    pl_ = pl[fi % 2]
                    pgl = "pgl%d" % (fi % 2)
                    for k in range(8):
                        p.mm(pg_[:, 0:w], wv[:, k, fc * 128:(fc + 1) * 128, 0], xT_[:, k, c_], k == 0, k == 7, wr + xr, [pgl])
                    for k in range(8):
                        p.mm(pl_[:, 0:w], wv[:, k, fc * 128:(fc + 1) * 128, 1], xT_[:, k, c_], k == 0, k == 7, wr + xr, [pgl])
                    b2 = fi % 2
                    glu_ = gluD[b2]
                    sig_ = sigD[b2]
                    lin_ = linD[b2]
                    p.ts("dve", glu_[:, 0:w], pg_[:, 0:w], bias[:, fc, e:e + 1], 7.0, ALU.add, ALU.min, [pgl, "bias"], ["glu%d" % b2])
                    p.act(sig_[:, 0:w], glu_[:, 0:w], AF.Sigmoid, ["glu%d" % b2], ["sig%d" % b2], scale=1.702)
                    p.ts("dve", lin_[:, 0:w], pl_[:, 0:w], bias[:, 8 + fc, e:e + 1], 8.0, ALU.add, ALU.min, [pgl, "bias"], ["lin%d" % b2])
                    if fin is not None:
                        fin()

                    def fin(glu_=glu_, sig_=sig_, lin_=lin_, b2=b2, fc=fc, hws=hws, c_=c_, w=w):
                        p.tt("dve", gs[:, 0:w], glu_[:, 0:w], sig_[:, 0:w], ALU.mult, ["glu%d" % b2, "sig%d" % b2], ["gs"])
                        p.stt(aT_[:, fc, c_], lin_[:, 0:w], -6.0, gs[:, 0:w], ALU.max, ALU.mult, ["gs", "lin%d" % b2],
                              [aTn + "_%d_%d" % (fc, hh) for hh in hws])
                fin()
                p.sc.cur_cond = None

        def stage_o(g):
            e, grp = groups[g]
            base = grp * 512
            aT_ = aT[g % 2]
            aTn = "aT%d" % (g % 2)
            o_ = wout[e % 2]
            wor = ["wout%d_%d" % (e % 2, q) for q in range(2)]
            for tt in range(4):
                p.sc.cur_cond = (e, base + tt * 128)
                s0 = e * CAP + base + tt * 128
                ar = [aTn + "_%d_%d" % (fc, tt // 2) for fc in range(8)]
                y_ = ysb[st["yi"] % 2]
                yn = "ysb%d" % (st["yi"] % 2)
                st["yi"] += 1
                for hf in range(2):
                    for fc in range(8):
                        p.mm(py[hf][:], aT_[:, fc, tt * 128:(tt + 1) * 128], o_[:, fc, hf * 512:(hf + 1) * 512], fc == 0, fc == 7, ar + wor, ["py%d" % hf])
                    p.tt("dve", y_[:, hf * 512:(hf + 1) * 512], py[hf][:], bout[e % 2][:, hf * 512:(hf + 1) * 512], ALU.add,
                         ["py%d" % hf, "bout%d" % (e % 2)], [yn + "_%d" % hf])
                p.dma("sp", T["YS"][s0:s0 + 128, :], y_[:], [yn + "_0", yn + "_1"], ["YS"])
                p.sc.cur_cond = None

        stage_l(0)
        for g, (e, grp) in enumerate(groups):
            if grp == 0 and e + 1 < NE:
                loadw(e + 1)
            stage_c(g)
            if g + 1 < len(groups):
                stage_l(g + 1)
            stage_o(g)
        p.sc.emit()


def phase_c(nc, T, layer):
    with nc.cleanup_on_exit(), ExitStack() as es:
        sb = lambda name, shape, dt: es.enter_context(nc.sbuf_tensor("u%d_" % _U[0] + name, shape, dt))
        p = Ph(nc)
        epst = sb("epst", [128, 1], F32)
        p.memset("pool", epst[:], EPS, ["eps"])
        p.eps_ap = epst[:, 0:1]
        g2 = sb("g2", [128, 3, 1024], F32)
        gfin = sb("gfin", [128, 1024], F32)
        _load_mod(p, g2, T, layer, 5, "g2")
        p.dma("sp", gfin[:], T["final_norm_g"].partition_broadcast(128), [], ["gfin"])
        yk = [[sb("yk%d_%d" % (i, k), [128, 1024], F32) for k in range(4)] for i in range(2)]
        xt = [sb("xt%d" % i, [128, 1024], F32) for i in range(2)]
        sl = [sb("sl%d" % i, [128, 4], I32) for i in range(2)]
        ga = [sb("ga%d" % i, [128, 4], F32) for i in range(2)]
        acc = sb("acc", [128, 1024], F32)
        t1 = sb("t1", [128, 1024], F32)
        xo = [sb("xo%d" % i, [128, 1024], F32) for i in range(2)]
        junk = sb("junk", [128, 1024], BF16)
        ss = sb("ss", [128, 48], F32)
        oo = [sb("oo%d" % i, [128, 1024], F32) for i in range(2)]
        ntl = NT if layer == 0 else 16
        tiles = [(b, t) for b in range(NBC) for t in range(ntl)]

        def load(it):
            b, t = tiles[it]
            rows = slice(t * 128, (t + 1) * 128)
            i2 = it % 2
            p.dma("sp", sl[i2][:], T["SLOT"][b, rows, :], [], ["sl%d" % i2])
            p.dma("sp", ga[i2][:], T["GATE"][b, rows, :], [], ["ga%d" % i2])
            p.dma("sp", xt[i2][:], T["XR"][b, rows, :], [], ["xt%d" % i2])
            for k in range(4):
                p.sc.dma("pool", lambda e, k=k, i2=i2: e.indirect_dma_start(
                    out=yk[i2][k][:], out_offset=None, in_=T["YS"],
                    in_offset=bass.IndirectOffsetOnAxis(ap=sl[i2][:, k:k + 1], axis=0)), ["sl%d" % i2], ["yk%d_%d" % (i2, k)])

        load(0)
        for it, (b, t) in enumerate(tiles):
            if it + 1 < len(tiles):
                load(it + 1)
            i2 = it % 2
            j = b if t < 16 else 2
            rows = slice(t * 128, (t + 1) * 128)
            ykr = ["yk%d_%d" % (i2, k) for k in range(4)]
            p.ts("dve", acc[:], yk[i2][0][:], ga[i2][:, 0:1], None, ALU.mult, None, [ykr[0], "ga%d" % i2], ["acc"])
            for k in range(1, 4):
                p.stt(acc[:], yk[i2][k][:], ga[i2][:, k:k + 1], acc[:], ALU.mult, ALU.add, [ykr[k], "ga%d" % i2, "acc"], ["acc"])
            p.tt("pool", t1[:], acc[:], g2[:, j, :], ALU.mult, ["acc", "g2"], ["t1"])
            x_ = xo[i2]
            xon = "xo%d" % i2
            p.tt("dve", x_[:], t1[:], xt[i2][:], ALU.add, ["t1", "xt%d" % i2], [xon])
            if layer == 0:
                p.dma("sp", T["XR"][b, rows, :], x_[:], [xon], ["XRw"])
            else:
                p.act(junk[:], x_[:], AF.Square, [xon], ["junk", "ss"], accum=ss[:, 0:1])
                p.rstd(ss, 1, "ss", 1024)
                p.stt(oo[i2][:], x_[:], ss[:, 32:33], gfin[:], ALU.mult, ALU.mult, [xon, "ss_r", "gfin"], ["oo%d" % i2])
                p.dma("sp", T["out"][b, rows, :], oo[i2][:], ["oo%d" % i2], ["outw"])
        p.sc.emit()


_IN_SHAPES = {
    "x": ([NBC, SEQ, D], F32), "ctx": ([NBC, NCTX, D], F32), "c": ([NBC, D], F32), "c_ctx": ([D], F32),
    "ada_w": ([2, D, 6 * D], F32), "ada_b": ([2, 6 * D], F32), "norm_mix_g": ([2, D], F32), "norm_ffn_g": ([2, D], F32),
    "ab_w_in": ([D, 1440], F32), "mla_q_norm_g": ([384], F32), "mla_wq_b": ([384, 768], F32),
    "mla_kv_norm_g": ([256], F32), "mla_wkv_b": ([256, 1024], F32), "swa_sink": ([8], F32),
    "ab_w_out": ([D, D], F32), "c_w_in": ([D, 1536], F32), "c_q_norm_g": ([128], F32), "c_k_norm_g": ([128], F32),
    "c_w_out": ([D, D], F32), "router_w": ([2, D, NE], F32), "router_b": ([2, NE], F32),
    "moe_w_in": ([2, NE, D, 2 * D], F32), "moe_b_in": ([2, NE, 2 * D], F32),
    "moe_w_out": ([2, NE, D, D], F32), "moe_b_out": ([2, NE, D], F32), "final_norm_g": ([D], F32),
    "IDENT": ([128, 128], F32), "MASKS": ([2, 128, 128], F32), "TRI": ([2, 128, 128], F32), "ECAP": ([128, NE], F32),
    "ROPE0": ([SEQ, 96], F32), "ROPE1": ([SEQ, 128], F32),
}
_SCRATCH = {
    "MOD": ([2, 3, 6 * D], F32), "XR": ([NBC, TOK, D], F32),
    "QAT": ([NBC, 128, 4, TOK], BF16), "KAT": ([NBC, 128, TOK], BF16), "VA1": ([NBC, TOK, 2, 65], BF16),
    "QBT": ([NBC, 96, 8, TOK], BF16), "KBT": ([NBC, 96, 8, TOK], BF16), "VB1": ([NBC, TOK, 8, 65], BF16),
    "QCT": ([NBC, 128, 8, SEQ], BF16), "KCT": ([NBC, 128, 2, TOK], BF16), "VC1": ([NBC, TOK, 2, 129], BF16),
    "OATT": ([NBC, TOK, D], BF16), "XS": ([NSLOT, D], BF16), "YS": ([NSLOT + 128, D], F32),
    "SLOT": ([NBC, TOK, 4], I32), "GATE": ([NBC, TOK, 4], F32), "CNT": ([2, 128, NE], F32), "CNTI": ([2, 1, NE], I32),
}


def build_program(stop_after=None, debug=()):
    nc = bass.Bass("TRN2", target_bir_lowering=False)
    T = {}
    for k, (shp, dt) in _IN_SHAPES.items():
        T[k] = nc.dram_tensor(k, shp, dt, kind="ExternalInput").ap()
    for k, (shp, dt) in _SCRATCH.items():
        kind = "ExternalOutput" if k in debug else "Internal"
        T[k] = nc.dram_tensor(k, shp, dt, kind=kind).ap()
    T["out"] = nc.dram_tensor("out", [NBC, SEQ, D], F32, kind="ExternalOutput").ap()
    phases = [
        ("mod", lambda: phase_mod(nc, T)),
        ("p1_0", lambda: phase_p1(nc, T, 0)),
        ("aa", lambda: phase_attn_a(nc, T)),
        ("ab", lambda: phase_attn_dense(nc, T, 0)),
        ("o_0", lambda: phase_o(nc, T, 0)),
        ("e_0", lambda: phase_e(nc, T, 0)),
        ("c_0", lambda: phase_c(nc, T, 0)),
        ("p1_1", lambda: phase_p1(nc, T, 1)),
        ("ac", lambda: phase_attn_dense(nc, T, 1)),
        ("o_1", lambda: phase_o(nc, T, 1)),
        ("e_1", lambda: phase_e(nc, T, 1)),
        ("c_1", lambda: phase_c(nc, T, 1)),
    ]
    for name, fn in phases:
        fn()
        if stop_after == name:
            break
    return nc


def _rope_tab(rot_dim):
    t = np.arange(SEQ)
    rows = (t // 64).astype(np.float32)
    cols = (t % 64).astype(np.float32)
    quarter = rot_dim // 4
    inv = (np.float32(10000.0) ** (-np.arange(quarter, dtype=np.float32) / np.float32(quarter))).astype(np.float32)
    ang = np.concatenate([rows[:, None] * inv, cols[:, None] * inv], axis=-1).astype(np.float32)
    return np.cos(ang).astype(np.float32), np.sin(ang).astype(np.float32)


def _const_inputs():
    ca, sa = _rope_tab(64)
    cb, sb_ = _rope_tab(32)
    cc, sc_ = _rope_tab(128)
    pi = np.arange(128)
    masks = np.stack([(pi[:, None] >= pi[None, :]), (pi[:, None] <= pi[None, :])]).astype(np.float32)
    tri = np.stack([(pi[:, None] < pi[None, :]), np.ones((128, 128), bool)]).astype(np.float32)
    ecap = np.broadcast_to((np.arange(NE) * CAP).astype(np.float32)[None, :], (128, NE)).copy()
    return {
        "IDENT": np.eye(128, dtype=np.float32), "MASKS": masks, "TRI": tri, "ECAP": ecap,
        "ROPE0": np.ascontiguousarray(np.concatenate([ca, sa, cb, sb_], axis=1)),
        "ROPE1": np.ascontiguousarray(np.concatenate([cc, sc_], axis=1)),
    }


def make_in_maps(inputs, n_cores=8):
    consts = _const_inputs()
    sq = {"ab_w_in", "mla_q_norm_g", "mla_wq_b", "mla_kv_norm_g", "mla_wkv_b", "swa_sink", "ab_w_out",
          "c_w_in", "c_q_norm_g", "c_k_norm_g", "c_w_out"}
    shared = {}
    for k in _IN_SHAPES:
        if k in consts:
            shared[k] = consts[k]
        elif k in ("x", "ctx", "c"):
            continue
        else:
            a = np.asarray(inputs[k], dtype=np.float32)
            if k in sq:
                a = a[0]
            shared[k] = np.ascontiguousarray(a)
    maps = []
    for c in range(n_cores):
        m = dict(shared)
        for k in ("x", "ctx", "c"):
            m[k] = np.ascontiguousarray(np.asarray(inputs[k], dtype=np.float32)[c * NBC:(c + 1) * NBC])
        maps.append(m)
    return maps


def kernel(**inputs):
    nc = build_program()
    maps = make_in_maps(inputs, 8)
    res = run_bass_kernel_spmd(nc, maps, core_ids=list(range(8)))
    return np.concatenate([np.asarray(r["out"]) for r in res.results], axis=0).astype(np.float32)
```
